# Optimizing a Trainium2 kernel written in Bass

```python
import math
import jax
import jax.numpy as jnp
from jax import lax
import numpy as np

D_MODEL = 2048
BATCH = 4
SEQ = 4096
DEPTH = 2

D_MIX = D_MODEL
GROUP_W = D_MIX // 4
ROPE_THETA = 500000.0
NORM_EPS = 1e-6
Q_BLOCK = 128

MLA_HEADS = 4
MLA_NOPE = 128
MLA_ROPE = 64
MLA_V = 128
MLA_Q_RANK = 384
MLA_KV_RANK = 256

S5_CH = GROUP_W
S5_GROUP = 16
S5_NGROUPS = S5_CH // S5_GROUP
S5_STATE = 64

NSA_HEADS = 4
NSA_DIM = 128
NSA_ROT = NSA_DIM // 4
CMP_BLOCK = 32
CMP_STRIDE = 16
SEL_BLOCK = 64
SEL_TOPK = 16
WINDOW = 512

GDN_HEADS = 4
GDN_DIM = 128
GDN_CONV = 4
GDN_CHUNK = 64

D_FF = 5632
FFN_CONV = 3

IN_SIZES = (MLA_Q_RANK, MLA_KV_RANK, MLA_ROPE,
            S5_CH,
            NSA_HEADS * NSA_DIM, NSA_DIM, NSA_DIM, NSA_DIM, NSA_DIM, NSA_DIM, NSA_DIM, 3 * NSA_HEADS,
            GDN_HEADS * GDN_DIM, GDN_HEADS * GDN_DIM, GDN_HEADS * GDN_DIM, GDN_HEADS * GDN_DIM, GDN_HEADS, GDN_HEADS)
IN_COLS = sum(IN_SIZES)
MIX_OUT = MLA_HEADS * MLA_V + S5_CH + NSA_HEADS * NSA_DIM + GDN_HEADS * GDN_DIM

kernel_name = 'hymba_parallel_mla_s5_nsa_gdn_convffn'


def rms_norm(x, g):
    xf = x.astype(jnp.float32)
    y = xf * lax.rsqrt(jnp.mean(xf * xf, axis=-1, keepdims=True) + NORM_EPS)
    return (y * g.astype(jnp.float32)).astype(x.dtype)


def l2_norm(x):
    return x * lax.rsqrt(jnp.sum(x * x, axis=-1, keepdims=True) + 1e-6)


def rope(x, pos, rot_dim):
    half = rot_dim // 2
    inv_freq = ROPE_THETA ** (-jnp.arange(half, dtype=jnp.float32) / half)
    ang = pos.astype(jnp.float32)[:, :, None] * inv_freq
    cos = jnp.cos(ang)[:, :, None, :]
    sin = jnp.sin(ang)[:, :, None, :]
    xr = x[..., :rot_dim].astype(jnp.float32)
    x1, x2 = xr[..., :half], xr[..., half:]
    rot = jnp.concatenate([x1 * cos - x2 * sin, x2 * cos + x1 * sin], axis=-1).astype(x.dtype)
    return jnp.concatenate([rot, x[..., rot_dim:]], axis=-1)


def causal_dwconv(x, w):
    k = w.shape[0]
    return lax.conv_general_dilated(x, w[:, None, :].astype(x.dtype), window_strides=(1,),
                                    padding=[(k - 1, 0)], dimension_numbers=('NWC', 'WIO', 'NWC'),
                                    feature_group_count=x.shape[-1])


def split_cols(x, sizes):
    return jnp.split(x, np.cumsum(sizes)[:-1].tolist(), axis=-1)


def blocked_causal_attention(q, k, v, scale):
    B, S, H, Dk = q.shape
    nb = S // Q_BLOCK
    qb = q.reshape(B, nb, Q_BLOCK, H, Dk).swapaxes(0, 1)
    kpos = jnp.arange(S)

    def one(args):
        i, qi = args
        s = jnp.einsum('bqhd,bkhd->bhqk', qi, k).astype(jnp.float32) * scale
        qpos = i * Q_BLOCK + jnp.arange(Q_BLOCK)
        s = jnp.where(kpos[None, :] <= qpos[:, None], s, -jnp.inf)
        p = jax.nn.softmax(s, axis=-1).astype(v.dtype)
        return jnp.einsum('bhqk,bkhd->bqhd', p, v)

    o = lax.map(one, (jnp.arange(nb), qb))
    return o.swapaxes(0, 1).reshape(B, S, H, v.shape[-1])


def mla_mixer(cq, ckv, kpe, pos, q_norm, w_uq, kv_norm, w_ukv):
    B, S, _ = cq.shape
    q = (rms_norm(cq, q_norm) @ w_uq).reshape(B, S, MLA_HEADS, MLA_NOPE + MLA_ROPE)
    kv = (rms_norm(ckv, kv_norm) @ w_ukv).reshape(B, S, MLA_HEADS, MLA_NOPE + MLA_V)
    q = jnp.concatenate([q[..., :MLA_NOPE], rope(q[..., MLA_NOPE:], pos, MLA_ROPE)], axis=-1)
    k_pe = rope(kpe[:, :, None, :], pos, MLA_ROPE)
    k = jnp.concatenate([kv[..., :MLA_NOPE], jnp.broadcast_to(k_pe, (B, S, MLA_HEADS, MLA_ROPE))], axis=-1)
    o = blocked_causal_attention(q, k, kv[..., MLA_NOPE:], (MLA_NOPE + MLA_ROPE) ** -0.5)
    return o.reshape(B, S, MLA_HEADS * MLA_V)


def s5_mixer(u, a_re, a_im, b_re, b_im, c_re, c_im, d_skip, log_step, w_glu, b_glu):
    B, S, _ = u.shape
    f32 = jnp.float32
    uf = u.astype(f32).reshape(B, S, S5_NGROUPS, S5_GROUP)
    step = jnp.exp(log_step.astype(f32))[:, None]
    are, aim = a_re.astype(f32), a_im.astype(f32)
    mag = jnp.exp(are * step)
    lb_re, lb_im = mag * jnp.cos(aim * step), mag * jnp.sin(aim * step)
    den = are * are + aim * aim
    nr, ni = lb_re - 1.0, lb_im
    g_re = (nr * are + ni * aim) / den
    g_im = (ni * are - nr * aim) / den
    br, bi = b_re.astype(f32), b_im.astype(f32)
    bb_re = g_re[..., None] * br - g_im[..., None] * bi
    bb_im = g_re[..., None] * bi + g_im[..., None] * br
    bu_re = jnp.einsum('bsgc,gpc->bsgp', uf, bb_re)
    bu_im = jnp.einsum('bsgc,gpc->bsgp', uf, bb_im)
    lam_re = jnp.broadcast_to(lb_re, bu_re.shape)
    lam_im = jnp.broadcast_to(lb_im, bu_re.shape)

    def combine(e1, e2):
        a1r, a1i, b1r, b1i = e1
        a2r, a2i, b2r, b2i = e2
        return (a2r * a1r - a2i * a1i, a2r * a1i + a2i * a1r,
                a2r * b1r - a2i * b1i + b2r, a2r * b1i + a2i * b1r + b2i)

    _, _, xr, xi = lax.associative_scan(combine, (lam_re, lam_im, bu_re, bu_im), axis=1)
    y = jnp.einsum('bsgp,gcp->bsgc', xr, c_re.astype(f32)) - jnp.einsum('bsgp,gcp->bsgc', xi, c_im.astype(f32))
    y = y.reshape(B, S, S5_CH) + d_skip.astype(f32) * u.astype(f32)
    y = jax.nn.gelu(y).astype(u.dtype)
    y = y * jax.nn.sigmoid(y @ w_glu + b_glu)
    return y


def compress_blocks(xb, pos_emb, w1, w2):
    B, N, L, D = xb.shape
    h = jax.nn.gelu((xb + pos_emb).reshape(B, N, L * D) @ w1)
    return h @ w2


def nsa_compressed(q, kc, vc, pos_k, w1k, w2k, pos_v, w1v, w2v, scale):
    B, S, H, D = q.shape
    n_cmp = (S - CMP_BLOCK) // CMP_STRIDE + 1
    idx = np.arange(n_cmp)[:, None] * CMP_STRIDE + np.arange(CMP_BLOCK)[None, :]
    k_cmp = compress_blocks(kc[:, idx], pos_k, w1k, w2k)
    v_cmp = compress_blocks(vc[:, idx], pos_v, w1v, w2v)
    s = jnp.einsum('bshd,bnd->bhsn', q, k_cmp).astype(jnp.float32) * scale
    blk_end = np.arange(n_cmp) * CMP_STRIDE + CMP_BLOCK - 1
    valid = jnp.arange(S)[:, None] >= blk_end[None, :]
    p = jax.nn.softmax(jnp.where(valid, s, -1e30), axis=-1)
    p = jnp.where(valid, p, 0.0)
    o = jnp.einsum('bhsn,bnd->bshd', p.astype(v_cmp.dtype), v_cmp)
    return o, p


def nsa_select_blocks(p_cmp):
    B, H, S, n_cmp = p_cmp.shape
    n_sel = S // SEL_BLOCK
    sel_start = np.arange(n_sel) * SEL_BLOCK
    cmp_start = np.arange(n_cmp) * CMP_STRIDE
    overlap = ((cmp_start[:, None] < sel_start[None, :] + SEL_BLOCK)
               & (cmp_start[:, None] + CMP_BLOCK > sel_start[None, :])).astype(np.float32)
    imp = jnp.einsum('bhsn,nj->bsj', p_cmp, jnp.asarray(overlap))
    t_blk = jnp.arange(S) // SEL_BLOCK
    j = jnp.arange(n_sel)
    forced = (j[None, :] == 0) | (j[None, :] == t_blk[:, None]) | (j[None, :] == t_blk[:, None] - 1)
    future = j[None, :] > t_blk[:, None]
    imp = jnp.where(forced, 1e6, jnp.where(future, -1e6, imp))
    _, sel_idx = lax.top_k(imp, min(SEL_TOPK, n_sel))
    return sel_idx


def nsa_selected(q, ks, vs, sel_idx, scale):
    B, S, H, D = q.shape
    n_sel = S // SEL_BLOCK
    nb = S // Q_BLOCK
    n_top = sel_idx.shape[-1]
    ks_b = ks.reshape(B, n_sel, SEL_BLOCK, D)
    vs_b = vs.reshape(B, n_sel, SEL_BLOCK, D)
    qb = q.reshape(B, nb, Q_BLOCK, H, D).swapaxes(0, 1)
    ib = sel_idx.reshape(B, nb, Q_BLOCK, n_top).swapaxes(0, 1)
    bidx = jnp.arange(B)[:, None, None]

    def one(args):
        i, qi, ii = args
        kg = ks_b[bidx, ii]
        vg = vs_b[bidx, ii]
        s = jnp.einsum('bqhd,bqkld->bhqkl', qi, kg).astype(jnp.float32) * scale
        kpos = ii[..., None] * SEL_BLOCK + jnp.arange(SEL_BLOCK)
        qpos = i * Q_BLOCK + jnp.arange(Q_BLOCK)
        mask = kpos <= qpos[None, :, None, None]
        s = jnp.where(mask[:, None], s, -jnp.inf)
        p = jax.nn.softmax(s.reshape(B, H, Q_BLOCK, n_top * SEL_BLOCK), axis=-1).reshape(s.shape)
        return jnp.einsum('bhqkl,bqkld->bqhd', p.astype(vg.dtype), vg)

    o = lax.map(one, (jnp.arange(nb), qb, ib))
    return o.swapaxes(0, 1).reshape(B, S, H, D)


def nsa_window(q, kw, vw, scale):
    B, S, H, D = q.shape
    nb = S // Q_BLOCK
    kp = jnp.pad(kw, ((0, 0), (WINDOW, 0), (0, 0)))
    vp = jnp.pad(vw, ((0, 0), (WINDOW, 0), (0, 0)))
    qb = q.reshape(B, nb, Q_BLOCK, H, D).swapaxes(0, 1)

    def one(args):
        i, qi = args
        kb = lax.dynamic_slice_in_dim(kp, i * Q_BLOCK, WINDOW + Q_BLOCK, axis=1)
        vb = lax.dynamic_slice_in_dim(vp, i * Q_BLOCK, WINDOW + Q_BLOCK, axis=1)
        s = jnp.einsum('bqhd,bkd->bhqk', qi, kb).astype(jnp.float32) * scale
        kpos = i * Q_BLOCK - WINDOW + jnp.arange(WINDOW + Q_BLOCK)
        qpos = i * Q_BLOCK + jnp.arange(Q_BLOCK)
        rel = qpos[:, None] - kpos[None, :]
        mask = (rel >= 0) & (rel < WINDOW) & (kpos[None, :] >= 0)
        p = jax.nn.softmax(jnp.where(mask, s, -jnp.inf), axis=-1)
        return jnp.einsum('bhqk,bkd->bqhd', p.astype(vb.dtype), vb)

    o = lax.map(one, (jnp.arange(nb), qb))
    return o.swapaxes(0, 1).reshape(B, S, H, D)


def nsa_mixer(nq, kc, vc, ks, vs, kw, vw, ngate, pos, pos_k, w1k, w2k, pos_v, w1v, w2v):
    B, S, _ = nq.shape
    scale = NSA_DIM ** -0.5
    q = rope(nq.reshape(B, S, NSA_HEADS, NSA_DIM), pos, NSA_ROT)

    def rot_k(t):
        return rope(t[:, :, None, :], pos, NSA_ROT)[:, :, 0, :]

    o_cmp, p_cmp = nsa_compressed(q, rot_k(kc), vc, pos_k, w1k, w2k, pos_v, w1v, w2v, scale)
    sel_idx = nsa_select_blocks(p_cmp)
    o_slc = nsa_selected(q, rot_k(ks), vs, sel_idx, scale)
    o_win = nsa_window(q, rot_k(kw), vw, scale)
    gates = jax.nn.sigmoid(ngate).reshape(B, S, 3, NSA_HEADS, 1)
    o = gates[:, :, 0] * o_cmp + gates[:, :, 1] * o_slc + gates[:, :, 2] * o_win
    return o.reshape(B, S, NSA_HEADS * NSA_DIM)


def chunked_gated_delta_rule(q, k, v, g, beta):
    B, S, H, D = q.shape
    C = GDN_CHUNK
    N = S // C

    def chunks(t):
        return t.reshape(B, N, C, H, -1).transpose(1, 0, 3, 2, 4)

    qc, kc, vc = chunks(q), chunks(k), chunks(v)
    gc = jnp.cumsum(g.reshape(B, N, C, H).transpose(1, 0, 3, 2), axis=-1)
    bc = beta.reshape(B, N, C, H).transpose(1, 0, 3, 2)
    incl = jnp.tril(jnp.ones((C, C), dtype=bool))
    strict = jnp.tril(jnp.ones((C, C), dtype=bool), -1)
    diff = gc[..., :, None] - gc[..., None, :]
    decay = jnp.where(incl, jnp.exp(jnp.where(incl, diff, 0.0)), 0.0)
    k_beta = kc * bc[..., None]
    v_beta = vc * bc[..., None]
    lower = jnp.where(strict, jnp.einsum('nbhid,nbhjd->nbhij', k_beta, kc) * decay, 0.0)
    eye = jnp.eye(C, dtype=q.dtype)
    t_inv = lax.linalg.triangular_solve(eye + lower, jnp.broadcast_to(eye, lower.shape),
                                        left_side=True, lower=True)
    u = t_inv @ v_beta
    w = t_inv @ (k_beta * jnp.exp(gc)[..., None])
    intra = jnp.einsum('nbhid,nbhjd->nbhij', qc, kc) * decay
    q_dec = qc * jnp.exp(gc)[..., None]
    g_last = gc[..., -1]
    k_dec = kc * jnp.exp(g_last[..., None] - gc)[..., None]

    def step(state, xs):
        q_i, u_i, w_i, a_i, k_i, gl_i = xs
        v_new = u_i - w_i @ state
        o_i = q_i @ state + a_i @ v_new
        state = state * jnp.exp(gl_i)[..., None, None] + jnp.swapaxes(k_i, -1, -2) @ v_new
        return state, o_i

    state0 = jnp.zeros((B, H, D, D), q.dtype)
    _, o = lax.scan(step, state0, (q_dec, u, w, intra, k_dec, g_last))
    return o.transpose(1, 0, 3, 2, 4).reshape(B, S, H, D)


def gdn_mixer(gq, gk, gv, gz, ga, gb, conv_w, a_log, dt_bias, o_norm):
    B, S, _ = gq.shape
    f32 = jnp.float32
    qkv = jax.nn.silu(causal_dwconv(jnp.concatenate([gq, gk, gv], axis=-1), conv_w))
    q, k, v = [t.reshape(B, S, GDN_HEADS, GDN_DIM).astype(f32) for t in jnp.split(qkv, 3, axis=-1)]
    q = l2_norm(q) * GDN_DIM ** -0.5
    k = l2_norm(k)
    beta = jax.nn.sigmoid(gb.astype(f32))
    g = -jnp.exp(a_log.astype(f32)) * jax.nn.softplus(ga.astype(f32) + dt_bias.astype(f32))
    o = chunked_gated_delta_rule(q, k, v, g, beta)
    o = rms_norm(o, o_norm) * jax.nn.silu(gz.reshape(B, S, GDN_HEADS, GDN_DIM).astype(f32))
    return o.reshape(B, S, GDN_HEADS * GDN_DIM).astype(gq.dtype)


def token_mixing(h, pos, w_in, w_out, gn_mla, gn_s5, gn_nsa,
                 mla_q_norm, mla_w_uq, mla_kv_norm, mla_w_ukv,
                 s5_a_re, s5_a_im, s5_b_re, s5_b_im, s5_c_re, s5_c_im, s5_d, s5_log_step, s5_w_glu, s5_b_glu,
                 nsa_pos_k, nsa_w1_k, nsa_w2_k, nsa_pos_v, nsa_w1_v, nsa_w2_v,
                 gdn_conv_w, gdn_a_log, gdn_dt_bias, gdn_o_norm):
    proj = h @ w_in
    (cq, ckv, kpe, u_s5, nq, kc, vc, ks, vs, kw, vw, ngate,
     gq, gk, gv, gz, ga, gb) = split_cols(proj, IN_SIZES)
    o_a = mla_mixer(cq, ckv, kpe, pos, mla_q_norm, mla_w_uq, mla_kv_norm, mla_w_ukv)
    o_b = s5_mixer(u_s5, s5_a_re, s5_a_im, s5_b_re, s5_b_im, s5_c_re, s5_c_im, s5_d, s5_log_step,
                   s5_w_glu, s5_b_glu)
    o_c = nsa_mixer(nq, kc, vc, ks, vs, kw, vw, ngate, pos, nsa_pos_k, nsa_w1_k, nsa_w2_k,
                    nsa_pos_v, nsa_w1_v, nsa_w2_v)
    o_d = gdn_mixer(gq, gk, gv, gz, ga, gb, gdn_conv_w, gdn_a_log, gdn_dt_bias, gdn_o_norm)
    o = jnp.concatenate([rms_norm(o_a, gn_mla), rms_norm(o_b, gn_s5), rms_norm(o_c, gn_nsa), o_d], axis=-1)
    return o @ w_out


def conv_ffn(h, w_in, conv_w, w_out):
    gate, val = jnp.split(h @ w_in, 2, axis=-1)
    gate = causal_dwconv(gate, conv_w)
    return (jax.nn.gelu(gate) * val) @ w_out


def setup_inputs(seed: int = 0) -> dict:
    key = jax.random.key(seed)
    keys = jax.random.split(key, 64)
    counter = [0]
    f32 = jnp.float32
    L = DEPTH

    def nk():
        counter[0] += 1
        return keys[counter[0] - 1]

    def nrm(shape, scale):
        return jax.random.normal(nk(), shape, f32) * scale

    def gain(width):
        return 1.0 + nrm((L, width), 0.02)

    x = nrm((BATCH, SEQ, D_MODEL), 1.0)
    c = nrm((BATCH, D_MODEL), 1.0)
    positions = (jnp.arange(SEQ, dtype=jnp.int32)[None, :]
                 + jax.random.randint(nk(), (BATCH, 1), 0, 1024, dtype=jnp.int32))
    w_ada = nrm((L, D_MODEL, 6 * D_MODEL), D_MODEL ** -0.5)
    b_ada = nrm((L, 6 * D_MODEL), 0.01)
    norm_pre_mix = gain(D_MODEL)
    norm_post_mix = gain(D_MODEL)
    norm_pre_ffn = gain(D_MODEL)
    norm_post_ffn = gain(D_MODEL)
    w_in = nrm((L, D_MODEL, IN_COLS), D_MODEL ** -0.5)
    w_out = nrm((L, MIX_OUT, D_MODEL), MIX_OUT ** -0.5)
    gn_mla = gain(MLA_HEADS * MLA_V)
    gn_s5 = gain(S5_CH)
    gn_nsa = gain(NSA_HEADS * NSA_DIM)
    mla_q_norm = gain(MLA_Q_RANK)
    mla_w_uq = nrm((L, MLA_Q_RANK, MLA_HEADS * (MLA_NOPE + MLA_ROPE)), MLA_Q_RANK ** -0.5)
    mla_kv_norm = gain(MLA_KV_RANK)
    mla_w_ukv = nrm((L, MLA_KV_RANK, MLA_HEADS * (MLA_NOPE + MLA_V)), MLA_KV_RANK ** -0.5)
    s5_a_re = -0.5 + nrm((L, S5_NGROUPS, S5_STATE), 0.01)
    s5_a_im = math.pi * jnp.arange(S5_STATE, dtype=f32) + nrm((L, S5_NGROUPS, S5_STATE), 0.01)
    s5_b_re = nrm((L, S5_NGROUPS, S5_STATE, S5_GROUP), 0.5 ** 0.5)
    s5_b_im = nrm((L, S5_NGROUPS, S5_STATE, S5_GROUP), 0.5 ** 0.5)
    s5_c_re = nrm((L, S5_NGROUPS, S5_GROUP, S5_STATE), (2 * S5_STATE) ** -0.5)
    s5_c_im = nrm((L, S5_NGROUPS, S5_GROUP, S5_STATE), (2 * S5_STATE) ** -0.5)
    s5_d = nrm((L, S5_CH), 1.0)
    s5_log_step = jax.random.uniform(nk(), (L, S5_NGROUPS), f32, math.log(1e-3), math.log(1e-1))
    s5_w_glu = nrm((L, S5_CH, S5_CH), S5_CH ** -0.5)
    s5_b_glu = nrm((L, S5_CH), 0.01)
    nsa_pos_k = nrm((L, CMP_BLOCK, NSA_DIM), 0.02)
    nsa_w1_k = nrm((L, CMP_BLOCK * NSA_DIM, NSA_DIM), (CMP_BLOCK * NSA_DIM) ** -0.5)
    nsa_w2_k = nrm((L, NSA_DIM, NSA_DIM), NSA_DIM ** -0.5)
    nsa_pos_v = nrm((L, CMP_BLOCK, NSA_DIM), 0.02)
    nsa_w1_v = nrm((L, CMP_BLOCK * NSA_DIM, NSA_DIM), (CMP_BLOCK * NSA_DIM) ** -0.5)
    nsa_w2_v = nrm((L, NSA_DIM, NSA_DIM), NSA_DIM ** -0.5)
    gdn_conv_w = nrm((L, GDN_CONV, 3 * GDN_HEADS * GDN_DIM), GDN_CONV ** -0.5)
    gdn_a_log = jnp.log(jax.random.uniform(nk(), (L, GDN_HEADS), f32, 1.0, 16.0))
    dt = jnp.exp(jax.random.uniform(nk(), (L, GDN_HEADS), f32, math.log(1e-3), math.log(1e-1)))
    gdn_dt_bias = dt + jnp.log(-jnp.expm1(-dt))
    gdn_o_norm = gain(GDN_DIM)
    ffn_w_in = nrm((L, D_MODEL, 2 * D_FF), D_MODEL ** -0.5)
    ffn_conv_w = nrm((L, FFN_CONV, D_FF), FFN_CONV ** -0.5)
    ffn_w_out = nrm((L, D_FF, D_MODEL), D_FF ** -0.5)
    return {'x': x, 'c': c, 'positions': positions, 'w_ada': w_ada, 'b_ada': b_ada,
            'norm_pre_mix': norm_pre_mix, 'norm_post_mix': norm_post_mix,
            'norm_pre_ffn': norm_pre_ffn, 'norm_post_ffn': norm_post_ffn,
            'w_in': w_in, 'w_out': w_out, 'gn_mla': gn_mla, 'gn_s5': gn_s5, 'gn_nsa': gn_nsa,
            'mla_q_norm': mla_q_norm, 'mla_w_uq': mla_w_uq, 'mla_kv_norm': mla_kv_norm, 'mla_w_ukv': mla_w_ukv,
            's5_a_re': s5_a_re, 's5_a_im': s5_a_im, 's5_b_re': s5_b_re, 's5_b_im': s5_b_im,
            's5_c_re': s5_c_re, 's5_c_im': s5_c_im, 's5_d': s5_d, 's5_log_step': s5_log_step,
            's5_w_glu': s5_w_glu, 's5_b_glu': s5_b_glu,
            'nsa_pos_k': nsa_pos_k, 'nsa_w1_k': nsa_w1_k, 'nsa_w2_k': nsa_w2_k,
            'nsa_pos_v': nsa_pos_v, 'nsa_w1_v': nsa_w1_v, 'nsa_w2_v': nsa_w2_v,
            'gdn_conv_w': gdn_conv_w, 'gdn_a_log': gdn_a_log, 'gdn_dt_bias': gdn_dt_bias, 'gdn_o_norm': gdn_o_norm,
            'ffn_w_in': ffn_w_in, 'ffn_conv_w': ffn_conv_w, 'ffn_w_out': ffn_w_out}


def reference(x, c, positions, w_ada, b_ada, norm_pre_mix, norm_post_mix, norm_pre_ffn, norm_post_ffn,
              w_in, w_out, gn_mla, gn_s5, gn_nsa, mla_q_norm, mla_w_uq, mla_kv_norm, mla_w_ukv,
              s5_a_re, s5_a_im, s5_b_re, s5_b_im, s5_c_re, s5_c_im, s5_d, s5_log_step, s5_w_glu, s5_b_glu,
              nsa_pos_k, nsa_w1_k, nsa_w2_k, nsa_pos_v, nsa_w1_v, nsa_w2_v,
              gdn_conv_w, gdn_a_log, gdn_dt_bias, gdn_o_norm, ffn_w_in, ffn_conv_w, ffn_w_out):
    cond = jax.nn.silu(c)
    for l in range(DEPTH):
        mods = cond @ w_ada[l] + b_ada[l]
        sh1, sc1, g1, sh2, sc2, g2 = [m[:, None, :] for m in jnp.split(mods, 6, axis=-1)]
        h = rms_norm(x, norm_pre_mix[l]) * (1.0 + sc1) + sh1
        y = token_mixing(h, positions, w_in[l], w_out[l], gn_mla[l], gn_s5[l], gn_nsa[l],
                         mla_q_norm[l], mla_w_uq[l], mla_kv_norm[l], mla_w_ukv[l],
                         s5_a_re[l], s5_a_im[l], s5_b_re[l], s5_b_im[l], s5_c_re[l], s5_c_im[l],
                         s5_d[l], s5_log_step[l], s5_w_glu[l], s5_b_glu[l],
                         nsa_pos_k[l], nsa_w1_k[l], nsa_w2_k[l], nsa_pos_v[l], nsa_w1_v[l], nsa_w2_v[l],
                         gdn_conv_w[l], gdn_a_log[l], gdn_dt_bias[l], gdn_o_norm[l])
        x = x + g1 * rms_norm(y, norm_post_mix[l])
        h = rms_norm(x, norm_pre_ffn[l]) * (1.0 + sc2) + sh2
        y = conv_ffn(h, ffn_w_in[l], ffn_conv_w[l], ffn_w_out[l])
        x = x + g2 * rms_norm(y, norm_post_ffn[l])
    return x
```

```python
import numpy as np
from contextlib import ExitStack
import concourse.bass as bass
import concourse.mybir as mybir
from concourse.bass_utils import run_bass_kernel_spmd

F32 = mybir.dt.float32
BF16 = mybir.dt.bfloat16
I32 = mybir.dt.int32
AF = mybir.ActivationFunctionType
ALU = mybir.AluOpType
AX = mybir.AxisListType

D = 2048
S = 4096
NB = 4
DFF = 5632
EPS = 1e-6
NCH = 16


class Buf:
    __slots__ = ("w", "r", "name")

    def __init__(self, name=""):
        self.w = None
        self.r = []
        self.name = name


class Prog:
    NDMA = 24

    def __init__(self, nc):
        self.nc = nc
        self.es = ExitStack()
        self._root_es = self.es
        self.eng = {"pe": nc.tensor, "act": nc.scalar, "dve": nc.vector, "pool": nc.gpsimd, "sp": nc.sync}
        self.semh = {}
        self.skey = {}
        self.epoch = 0
        for e in self.eng:
            self.skey[e] = (e, 0)
            self.semh[(e, 0)] = self.es.enter_context(nc.semaphore("s_" + e))
        self.cnt = {e: 0 for e in self.eng}
        self.dsem = [self.es.enter_context(nc.semaphore("s_dma%d" % i)) for i in range(self.NDMA)]
        self.dcnt = [0] * self.NDMA
        self.dnext = 0
        self.dnext_sw = 0
        self.known = {e: {} for e in self.eng}
        self.nbuf = 0
        self.out_events = []

    def buf(self, name=""):
        return Buf(name)

    def sb(self, name, shape, dt):
        self.nbuf += 1
        name = "%s_%d" % (name, self.nbuf)
        t = self.es.enter_context(self.nc.sbuf_tensor(name, list(shape), dt))
        return t, Buf(name)

    def ps(self, name, shape, dt=F32):
        t = self.es.enter_context(self.nc.psum_tensor(name, list(shape), dt))
        return t, Buf(name)

    def _deps(self, e, reads, writes):
        need = {}

        def add(ev, same_ok):
            if ev is None:
                return
            sem_key, val, src = ev
            if src == e and same_ok:
                return
            if need.get(sem_key, 0) < val:
                need[sem_key] = val

        pe = (e == "pe")
        for b in reads:
            add(b.w, pe)
        for b in writes:
            add(b.w, pe)
            for r in b.r:
                add(r, pe)
        kn = self.known[e]
        for k, v in need.items():
            if kn.get(k, 0) < v:
                kn[k] = v
                semh = self.semh[k] if isinstance(k, tuple) else self.dsem[k]
                self.eng[e].wait_ge(semh, v)

    def op(self, e, fn, reads=(), writes=()):
        self._deps(e, reads, writes)
        ins = fn(self.eng[e])
        self.cnt[e] += 1
        ins.then_inc(self.semh[self.skey[e]], 1)
        ev = (self.skey[e], self.cnt[e], e)
        for b in reads:
            b.r.append(ev)
        for b in writes:
            b.w = ev
            b.r = []
        return ev

    def dma(self, out, in_, reads=(), writes=(), q="sp", is_output=False, **kw):
        self._deps(q, reads, writes)
        half = self.NDMA // 2
        if q == "pool":
            i = half + self.dnext_sw
            self.dnext_sw = (self.dnext_sw + 1) % (self.NDMA - half)
        else:
            i = self.dnext
            self.dnext = (self.dnext + 1) % half
        self.dcnt[i] += 16
        self.eng[q].dma_start(out=out, in_=in_, **kw).then_inc(self.dsem[i], 16)
        ev = (i, self.dcnt[i], "dma")
        for b in reads:
            b.r.append(ev)
        for b in writes:
            b.w = ev
            b.r = []
        if is_output:
            self.out_events.append(ev)
        return ev

    def push(self):
        self._outer = getattr(self, "_outer", [])
        self._outer.append(self.es)
        self.es = ExitStack()

    def pop(self):
        self.barrier()
        self.es.close()
        self.es = self._outer.pop()
        if max(self.cnt.values()) > 24000:
            self.epoch += 1
            for e in self.eng:
                if self.cnt[e]:
                    self.skey[e] = (e, self.epoch)
                    self.semh[(e, self.epoch)] = self._root_es.enter_context(
                        self.nc.semaphore("s_%s_%d" % (e, self.epoch)))
                    self.cnt[e] = 0

    def barrier(self):
        for e in self.eng:
            kn = self.known[e]
            for e2 in self.eng:
                k2 = self.skey[e2]
                if e2 != e and self.cnt[e2] > kn.get(k2, 0):
                    kn[k2] = self.cnt[e2]
                    self.eng[e].wait_ge(self.semh[k2], self.cnt[e2])
            for i in range(self.NDMA):
                if self.dcnt[i] > kn.get(i, 0):
                    kn[i] = self.dcnt[i]
                    self.eng[e].wait_ge(self.dsem[i], self.dcnt[i])

    def finish(self):
        for i in range(self.NDMA):
            if self.dcnt[i]:
                self.nc.sync.wait_ge(self.dsem[i], self.dcnt[i])
        for e in ("pe", "act", "dve", "pool"):
            if self.cnt[e]:
                self.nc.sync.wait_ge(self.semh[self.skey[e]], self.cnt[e])
        self.es.close()


def mm(P, out, lhsT, rhs, start, stop, reads, writes):
    return P.op("pe", lambda t: t.matmul(out, lhsT, rhs, start=start, stop=stop), reads, writes)


def act(P, out, in_, func, reads, writes, bias=None, scale=None, accum_out=None):
    kw = {}
    if bias is not None:
        kw["bias"] = bias
    if scale is not None:
        kw["scale"] = scale
    if accum_out is not None:
        kw["accum_out"] = accum_out
    return P.op("act", lambda a: a.activation(out=out, in_=in_, func=func, **kw), reads, writes)


def ts(P, out, in0, s1, s2, op0, op1, reads, writes, e="dve"):
    if op1 is None:
        return P.op(e, lambda v: v.tensor_scalar(out, in0, s1, None, op0), reads, writes)
    return P.op(e, lambda v: v.tensor_scalar(out, in0, s1, s2, op0, op1), reads, writes)


def stt(P, out, in0, scalar, in1, op0, op1, reads, writes, e="dve"):
    return P.op(e, lambda v: v.scalar_tensor_tensor(out, in0, scalar, in1, op0, op1), reads, writes)


def tt(P, out, in0, in1, op, reads, writes, e="dve"):
    return P.op(e, lambda v: v.tensor_tensor(out, in0, in1, op), reads, writes)


def splits(W):
    r = []
    o = 0
    while o < W:
        n = min(512, W - o)
        r.append((o, n))
        o += n
    return r


class PsumPool:
    def __init__(self, P, n=6):
        self.t = []
        for i in range(n):
            self.t.append(P.ps("psb%d" % i, [128, 512]))
        self.acc = [P.ps("psacc%d" % i, [128, 512]) for i in range(2)]
        self.i = 0
        self.n = n

    def get(self):
        t = self.t[self.i]
        self.i = (self.i + 1) % self.n
        return t


def rsqrt_(P, ap, B):
    act(P, ap, ap, AF.Sqrt, [B], [B])
    P.op("dve", lambda v: v.reciprocal(ap, ap), [B], [B])


def rstd_from_sumsq(P, rb, rbB, psum_ap, psB, n, dim, eps=EPS):
    ts(P, rb[:, :n], psum_ap, 1.0 / dim, eps, ALU.mult, ALU.add, [psB], [rbB])
    rsqrt_(P, rb[:, :n], rbB)


def phase_a(P, pp, io, NCOLCH, NTM):
    nc = P.nc
    ones_t, onesB = P.sb("a_ones", [128, 128], BF16)
    P.op("dve", lambda v: v.memset(ones_t[:], 1.0), [], [onesB])
    mods, modsB = P.sb("a_mods", [128, 96], F32)
    if "modsT_in" in io:
        P.dma(mods[:], io["modsT_in"], [], [modsB])
    else:
        P.push()
        cT, cB = P.sb("a_cT", [128, 16], F32)
        P.dma(cT[:], io["cT"], [], [cB])
        cond, condB = P.sb("a_cond", [128, 16], F32)
        act(P, cond[:], cT[:], AF.Silu, [cB], [condB])
        bada, badaB = P.sb("a_bada", [128, 96], F32)
        P.dma(bada[:], io["b_adaT"], [], [badaB])
        wa = [P.sb("a_wa%d" % i, [128, 16, 512], F32) for i in range(2)]
        pm, pmB = pp.get()
        for g in range(24):
            wt, wB = wa[g % 2]
            P.dma(wt[:], io["w_ada"][g], [], [wB], q="sp" if g % 2 == 0 else "act")
            for j in range(4):
                col = g * 4 + j
                for kc in range(16):
                    mm(P, pm[:, col:col + 1], wt[:, kc, j * 128:(j + 1) * 128], cond[:, kc:kc + 1],
                       kc == 0, kc == 15, [wB, condB], [pmB])
        tt(P, mods[:], pm[:, 0:96], bada[:], ALU.add, [pmB, badaB], [modsB])
        P.dma(io["modsT"], mods[:], [modsB], [], is_output=True)
        P.pop()
    n1, n1B = P.sb("a_n1", [128, 16], F32)
    P.dma(n1[:], io["norm1T"], [], [n1B])
    gsc, gscB = P.sb("a_gsc", [128, 16], F32)
    stt(P, gsc[:], mods[:, 16:32], 1.0, n1[:], ALU.add, ALU.mult, [modsB, n1B], [gscB])
    sh = mods[:, 0:16]

    xt, xB = P.sb("a_x", [128, 16, 512], F32)
    sq, sqB = P.sb("a_sq", [128, 16, 512], BF16)
    hT, hB = P.sb("a_hT", [128, 16, 2048], BF16)
    rb, rbB = P.sb("a_rb", [128, 512], F32)
    tmp, tmpB = P.sb("a_tmp", [128, 512], F32)
    wtm, wtmB = P.sb("a_wtm", [128, 16, NTM], BF16)
    P.dma(wtm[:], io["wtm"], [], [wtmB], q="pool")
    wbuf = [P.sb("a_w%d" % i, [128, 16, 128], BF16) for i in range(2)]
    obuf = [P.sb("a_o%d" % i, [128, 512], F32) for i in range(3)]
    xTv = io["xT"].rearrange("(kc p) t -> p kc t", p=128)
    oi = 0
    wi = 0
    for st in range(2):
        for tl in range(4):
            t0 = st * 2048 + tl * 512
            P.dma(xt[:], xTv[:, :, t0:t0 + 512], [], [xB])
            act(P, sq[:], xt[:], AF.Square, [xB], [sqB])
            ps, psB = pp.get()
            for kc in range(16):
                mm(P, ps[:], ones_t[:], sq[:, kc, :], kc == 0, kc == 15, [onesB, sqB], [psB])
            rstd_from_sumsq(P, rb, rbB, ps[:], psB, 512, D)
            for kc in range(16):
                stt(P, tmp[:], xt[:, kc, :], gsc[:, kc:kc + 1], rb[:], ALU.mult, ALU.mult,
                    [xB, gscB, rbB], [tmpB])
                act(P, hT[:, kc, tl * 512:(tl + 1) * 512], tmp[:], AF.Identity, [tmpB, modsB], [hB],
                    bias=sh[:, kc:kc + 1], scale=1.0)
        for c in range(NCOLCH):
            wt, wB = wbuf[wi % 2]
            wi += 1
            P.dma(wt[:], io["win"][c], [], [wB], q="pool")
            for tl in range(4):
                ps, psB = pp.get()
                for kc in range(16):
                    mm(P, ps[:], wt[:, kc, :], hT[:, kc, tl * 512:(tl + 1) * 512], kc == 0, kc == 15,
                       [wB, hB], [psB])
                ot, oB = obuf[oi % 3]
                oi += 1
                if oi % 2:
                    act(P, ot[:], ps[:], AF.Copy, [psB], [oB])
                else:
                    P.op("dve", lambda v, ot=ot, ps=ps: v.tensor_copy(ot[:], ps[:]), [psB], [oB])
                t0 = st * 2048 + tl * 512
                P.dma(io["proj"][c * 128:(c + 1) * 128, t0:t0 + 512], ot[:], [oB], [io["projB"]])
        for tk in range(16):
            ps, psB = pp.get()
            for kc in range(16):
                mm(P, ps[:, :NTM], hT[:, kc, tk * 128:(tk + 1) * 128], wtm[:, kc, :], kc == 0, kc == 15,
                   [wtmB, hB], [psB])
            ot, oB = obuf[oi % 3]
            oi += 1
            act(P, ot[:, :NTM], ps[:, :NTM], AF.Copy, [psB], [oB])
            t0 = st * 2048 + tk * 128
            P.dma(io["projT"][t0:t0 + 128, :], ot[:, :NTM], [oB], [io["projTB"]])


TB = 1024
WB = TB + 2


def phase_b(P, pp, io, ntiles=2):
    ones_t, onesB = P.sb("b_ones", [128, 128], BF16)
    P.op("dve", lambda v: v.memset(ones_t[:], 1.0), [], [onesB])
    mods, modsB = P.sb("b_mods", [128, 96], F32)
    P.dma(mods[:], io["modsT"], [], [modsB])
    vec, vecB = P.sb("b_vec", [128, 4, 16], F32)
    for i, k in enumerate(["gnT", "npostT", "npreT", "npost2T"]):
        P.dma(vec[:, i, :], io[k], [], [vecB])
    halo, haloB = P.sb("b_halo", [128, 1], F32)
    P.dma(halo[:], io["halo"], [], [haloB])
    cw, cwB = P.sb("b_cw", [128, 44, 3], F32)
    P.dma(cw[:], io["convT"], [], [cwB])
    der, derB = P.sb("b_der", [128, 3, 16], F32)
    tt(P, der[:, 0, :], mods[:, 32:48], vec[:, 1, :], ALU.mult, [modsB, vecB], [derB])
    stt(P, der[:, 1, :], mods[:, 64:80], 1.0, vec[:, 2, :], ALU.add, ALU.mult, [modsB, vecB], [derB])
    tt(P, der[:, 2, :], mods[:, 80:96], vec[:, 3, :], ALU.mult, [modsB, vecB], [derB])
    sh2 = mods[:, 48:64]
    gn = vec[:, 0, :]

    big, bigB = P.sb("b_big", [128, 16, WB], F32)
    bfa, bfaB = P.sb("b_bfa", [128, 16, WB], BF16)
    at, atB = P.sb("b_at", [128, 22, WB], BF16)
    stg, stgB = P.sb("b_stg", [128, 4, WB], F32)
    rb, rbB = P.sb("b_rb", [128, WB], F32)
    gbuf, gbB = P.sb("b_gbuf", [128, WB], F32)
    gtmp, gtB = P.sb("b_gtmp", [128, WB], F32)
    w1 = [P.sb("b_w1_%d" % i, [128, 16, 256], BF16) for i in range(2)]
    w2 = [P.sb("b_w2_%d" % i, [128, 22, 128], BF16) for i in range(2)]
    w1i = 0
    w2i = 0
    sp = splits(WB)
    oTv = io["oT"].rearrange("(kc p) t -> p kc t", p=128)
    xinv = io["xin"].rearrange("(kc p) t -> p kc t", p=128)
    xmidv = io["xmid"].rearrange("(kc p) t -> p kc t", p=128)
    xoutv = io["xout"].rearrange("(kc p) t -> p kc t", p=128)
    sqv = at

    def sumsq_rstd(src, srcB, nchunks, dim, c0=0):
        for c in range(nchunks):
            act(P, sqv[:, c, :], src[:, c0 + c, :], AF.Square, [srcB], [atB])
        for (o, n) in sp:
            ps, psB = pp.get()
            for c in range(nchunks):
                mm(P, ps[:, :n], ones_t[:], sqv[:, c, o:o + n], c == 0, c == nchunks - 1, [onesB, atB], [psB])
            ts(P, rb[:, o:o + n], ps[:, :n], 1.0 / dim, EPS, ALU.mult, ALU.add, [psB], [rbB])
        rsqrt_(P, rb[:], rbB)

    for tile in range(ntiles):
        c0 = tile * TB
        for g in range(4):
            P.dma(stg[:], oTv[:, 4 * g:4 * g + 4, c0:c0 + WB], [], [stgB])
            if g < 3:
                sumsq_rstd(stg, stgB, 4, 512)
                for c in range(4):
                    stt(P, bfa[:, 4 * g + c, :], stg[:, c, :], gn[:, 4 * g + c:4 * g + c + 1], rb[:],
                        ALU.mult, ALU.mult, [stgB, vecB, rbB], [bfaB])
            else:
                for c in range(4):
                    act(P, bfa[:, 4 * g + c, :], stg[:, c, :], AF.Copy, [stgB], [bfaB])
        for n_ in range(16):
            wt, wB = w2[w2i % 2]
            w2i += 1
            P.dma(wt[:, 0:16, :], io["wout"][n_], [], [wB], q="pool")
            for (o, n) in sp:
                ps, psB = pp.get()
                for kc in range(16):
                    mm(P, ps[:, :n], wt[:, kc, :], bfa[:, kc, o:o + n], kc == 0, kc == 15, [wB, bfaB], [psB])
                act(P, big[:, n_, o:o + n], ps[:, :n], AF.Copy, [psB], [bigB])
        sumsq_rstd(big, bigB, 16, D)
        for g in range(4):
            P.dma(stg[:], xinv[:, 4 * g:4 * g + 4, c0:c0 + WB], [], [stgB])
            for c in range(4):
                k = 4 * g + c
                tt(P, big[:, k, :], big[:, k, :], rb[:], ALU.mult, [bigB, rbB], [bigB])
                stt(P, big[:, k, :], big[:, k, :], der[:, 0, k:k + 1], stg[:, c, :], ALU.mult, ALU.add,
                    [bigB, derB, stgB], [bigB])
        sumsq_rstd(big, bigB, 16, D)
        for k in range(16):
            stt(P, gtmp[:], big[:, k, :], der[:, 1, k:k + 1], rb[:], ALU.mult, ALU.mult, [bigB, derB, rbB], [gtB])
            act(P, bfa[:, k, :], gtmp[:], AF.Identity, [gtB, modsB], [bfaB], bias=sh2[:, k:k + 1], scale=1.0)
        P.dma(xmidv, big[:], [bigB], [io["xmidB"]])
        for hh in range(2):
            for j in range(22):
                jj = hh * 22 + j
                wt, wB = w1[w1i % 2]
                w1i += 1
                P.dma(wt[:], io["wgv"][jj], [], [wB], q="pool")
                for (o, n) in sp:
                    ps, psB = pp.get()
                    for kc in range(16):
                        mm(P, ps[:, :n], wt[:, kc, 0:128], bfa[:, kc, o:o + n], kc == 0, kc == 15, [wB, bfaB], [psB])
                    act(P, gbuf[:, o:o + n], ps[:, :n], AF.Copy, [psB], [gbB])
                if tile == 0:
                    ts(P, gbuf[:, 0:2], gbuf[:, 0:2], halo[:, 0:1], None, ALU.mult, None, [gbB, haloB], [gbB])
                ts(P, gtmp[:, 2:WB], gbuf[:, 2:WB], cw[:, jj, 2:3], None, ALU.mult, None, [gbB, cwB], [gtB])
                stt(P, gtmp[:, 2:WB], gbuf[:, 1:WB - 1], cw[:, jj, 1:2], gtmp[:, 2:WB], ALU.mult, ALU.add,
                    [gbB, cwB, gtB], [gtB])
                stt(P, gtmp[:, 2:WB], gbuf[:, 0:WB - 2], cw[:, jj, 0:1], gtmp[:, 2:WB], ALU.mult, ALU.add,
                    [gbB, cwB, gtB], [gtB])
                act(P, gtmp[:, 2:WB], gtmp[:, 2:WB], AF.Gelu_apprx_tanh, [gtB], [gtB])
                for (o, n) in ((2, 512), (514, 512)):
                    ps, psB = pp.get()
                    for kc in range(16):
                        mm(P, ps[:, :n], wt[:, kc, 128:256], bfa[:, kc, o:o + n], kc == 0, kc == 15, [wB, bfaB], [psB])
                    tt(P, at[:, j, o:o + n], gtmp[:, o:o + n], ps[:, :n], ALU.mult, [gtB, psB], [atB])
            for n_ in range(16):
                wt, wB = w2[w2i % 2]
                w2i += 1
                P.dma(wt[:], io["wdn"][hh][n_], [], [wB], q="pool")
                for (o, n) in ((2, 512), (514, 512)):
                    ps, psB = pp.get()
                    for j in range(22):
                        mm(P, ps[:, :n], wt[:, j, :], at[:, j, o:o + n], j == 0, j == 21, [wB, atB], [psB])
                    if hh == 0:
                        act(P, big[:, n_, o:o + n], ps[:, :n], AF.Copy, [psB], [bigB])
                    else:
                        tt(P, big[:, n_, o:o + n], big[:, n_, o:o + n], ps[:, :n], ALU.add, [bigB, psB], [bigB])
        P.op("dve", lambda v: v.memset(big[:, :, 0:2], 0.0), [], [bigB])
        sumsq_rstd(big, bigB, 16, D)
        for g in range(4):
            P.dma(stg[:], xmidv[:, 4 * g:4 * g + 4, :], [io["xmidB"]], [stgB])
            for c in range(4):
                k = 4 * g + c
                tt(P, big[:, k, :], big[:, k, :], rb[:], ALU.mult, [bigB, rbB], [bigB])
                stt(P, stg[:, c, :], big[:, k, :], der[:, 2, k:k + 1], stg[:, c, :], ALU.mult, ALU.add,
                    [bigB, derB, stgB], [stgB])
            P.dma(xoutv[:, 4 * g:4 * g + 4, c0:c0 + TB], stg[:, :, 2:WB], [stgB], [], is_output=True)


OFF = dict(cq=0, ckv=384, kpe=640, u=704, nq=1216, kc=1728, vc=1856, ks=1984, vs=2112, kw=2240, vw=2368,
           ng=2496, gq=2508, gk=3020, gv=3532, gz=4044, ga=4556, gb=4560)
R_CQ, R_CKV, R_KPE, R_U, R_NQ, R_KC, R_VC, R_KS, R_KW, R_NG = 0, 384, 640, 704, 1216, 1728, 1856, 1984, 2112, 2240
R_GQ, R_GK, R_GV, R_GZ, R_GA, R_GB = 2248, 2504, 2760, 3016, 3272, 3274
NROWS = 3328
NCOLCH = NROWS // 128
NTM = 256


def col_plan(hf):
    cols = []
    cols += list(range(0, 704))
    for c in ([2 * hf, 2 * hf + 1, 2 * (1 - hf), 2 * (1 - hf) + 1]):
        cols += list(range(704 + 128 * c, 704 + 128 * c + 128))
    for h_ in ([2 * hf, 2 * hf + 1, 2 * (1 - hf), 2 * (1 - hf) + 1]):
        cols += list(range(1216 + 128 * h_, 1216 + 128 * h_ + 128))
    cols += list(range(OFF["kc"], OFF["kc"] + 128))
    cols += list(range(OFF["vc"], OFF["vc"] + 128))
    cols += list(range(OFF["ks"], OFF["ks"] + 128))
    cols += list(range(OFF["kw"], OFF["kw"] + 128))
    for r in range(3):
        for hh in range(2):
            cols.append(OFF["ng"] + r * 4 + 2 * hf + hh)
    cols += [-1, -1]
    for nm in ("gq", "gk", "gv", "gz"):
        cols += list(range(OFF[nm] + 256 * hf, OFF[nm] + 256 * hf + 256))
    cols += [OFF["ga"] + 2 * hf, OFF["ga"] + 2 * hf + 1, OFF["gb"] + 2 * hf, OFF["gb"] + 2 * hf + 1]
    cols += [-1] * (NROWS - len(cols))
    assert len(cols) == NROWS
    return np.array(cols)


def vecT(v):
    return np.ascontiguousarray(v.reshape(-1, 128).T)


def chunk_w(w, cols):
    K = w.shape[0]
    wz = np.concatenate([w, np.zeros((K, 1), w.dtype)], axis=1)
    sel = wz[:, cols]
    nch = len(cols) // 128
    return np.ascontiguousarray(sel.reshape(K // 128, 128, nch, 128).transpose(2, 1, 0, 3))


def host_phase_a_inputs(inp, l, b, hf):
    w_in = inp["w_in"][l]
    cols = col_plan(hf)
    tmcols = np.concatenate([np.arange(OFF["vs"], OFF["vs"] + 128), np.arange(OFF["vw"], OFF["vw"] + 128)])
    return {
        "xT": None,
        "norm1T": vecT(inp["norm_pre_mix"][l]),
        "win": chunk_w(w_in, cols),
        "wtm": np.ascontiguousarray(w_in[:, tmcols].reshape(16, 128, NTM).transpose(1, 0, 2)),
    }


def host_phase_b_inputs(inp, l):
    gn = np.concatenate([inp["gn_mla"][l], inp["gn_s5"][l], inp["gn_nsa"][l], np.ones(512, np.float32)])
    wf = inp["ffn_w_in"][l]
    gvcols = np.concatenate([np.stack([np.arange(j * 128, j * 128 + 128), DFF + np.arange(j * 128, j * 128 + 128)])
                             .reshape(-1) for j in range(44)])
    wdn = inp["ffn_w_out"][l]
    wdn_l = wdn.reshape(2, 22, 128, 16, 128).transpose(0, 3, 2, 1, 4)
    return {
        "gnT": vecT(gn), "npostT": vecT(inp["norm_post_mix"][l]), "npreT": vecT(inp["norm_pre_ffn"][l]),
        "npost2T": vecT(inp["norm_post_ffn"][l]),
        "wout": chunk_w(inp["w_out"][l], np.arange(D)),
        "wgv": chunk_w(wf, gvcols).reshape(44, 128, 16, 256) if False else
        np.ascontiguousarray(wf[:, gvcols].reshape(16, 128, 44, 256).transpose(2, 1, 0, 3)),
        "convT": np.ascontiguousarray(inp["ffn_conv_w"][l].reshape(3, 44, 128).transpose(2, 1, 0)),
        "wdn": np.ascontiguousarray(wdn_l),
    }


NEG = -30000.0


class Consts:
    def __init__(self, P):
        self.identb, self.identbB = P.sb("k_identb", [128, 128], BF16)
        self.identf, self.identfB = P.sb("k_identf", [128, 128], F32)
        self.onesb, self.onesbB = P.sb("k_onesb", [128, 128], BF16)
        self.onesf, self.onesfB = P.sb("k_onesf", [128, 128], F32)
        self.d0, self.d0B = P.sb("k_d0", [128, 512], F32)
        self.mask, self.maskB = P.sb("k_mask", [128, 12, 512], BF16)
        P.push()
        tmp, tmpB = P.sb("k_tmp", [128, 512], F32)
        tmp2, tmp2B = P.sb("k_tmp2", [128, 512], F32)
        P.op("dve", lambda v: v.memset(self.onesb[:], 1.0), [], [self.onesbB])
        P.op("dve", lambda v: v.memset(self.onesf[:], 1.0), [], [self.onesfB])
        P.op("pool", lambda g: g.iota(self.d0[:], pattern=[[1, 512]], base=0, channel_multiplier=-1,
                                      allow_small_or_imprecise_dtypes=True), [], [self.d0B])
        ts(P, self.identf[:], self.d0[:, 0:128], 0.0, None, ALU.is_equal, None, [self.d0B], [self.identfB])
        P.op("dve", lambda v: v.tensor_copy(self.identb[:], self.identf[:]), [self.identfB], [self.identbB])
        for moff in range(4):
            ts(P, tmp[:], self.d0[:], float(128 * moff), 0.0, ALU.subtract, ALU.is_ge, [self.d0B], [tmpB])
            ts(P, self.mask[:, moff, :], tmp[:], 1.0, -NEG, ALU.subtract, ALU.mult, [tmpB], [self.maskB])
        for i, moff in enumerate(range(-4, 4)):
            ts(P, tmp[:], self.d0[:], float(128 * moff), 0.0, ALU.subtract, ALU.is_ge, [self.d0B], [tmpB])
            ts(P, tmp2[:], self.d0[:], float(128 * moff), 512.0, ALU.subtract, ALU.is_lt, [self.d0B], [tmp2B])
            tt(P, tmp[:], tmp[:], tmp2[:], ALU.mult, [tmpB, tmp2B], [tmpB])
            ts(P, self.mask[:, 4 + i, :], tmp[:], 1.0, -NEG, ALU.subtract, ALU.mult, [tmpB], [self.maskB])
        P.pop()


def wrap_pi(P, x, xB, ki, kiB, kf, kfB):
    TWO_PI = float(2 * np.pi)
    ts(P, kf, x, 1.0 / TWO_PI, None, ALU.mult, None, [xB], [kfB])
    P.op("dve", lambda v: v.tensor_copy(ki, kf), [kfB], [kiB])
    P.op("dve", lambda v: v.tensor_copy(kf, ki), [kiB], [kfB])
    stt(P, x, kf, -TWO_PI, x, ALU.mult, ALU.add, [kfB, xB], [xB])
    ts(P, kf, x, float(np.pi), None, ALU.is_gt, None, [xB], [kfB])
    stt(P, x, kf, -TWO_PI, x, ALU.mult, ALU.add, [kfB, xB], [xB])
    ts(P, kf, x, -float(np.pi), None, ALU.is_lt, None, [xB], [kfB])
    stt(P, x, kf, TWO_PI, x, ALU.mult, ALU.add, [kfB, xB], [xB])


def rope_tables(P, pos_ap, nrow, half, C2, S2, tB, width):
    THETA = 500000.0
    P.push()
    posi, posiB = P.sb("r_posi", [128, S], I32)
    posf, posfB = P.sb("r_posf", [128, S], F32)
    r = 2 * half
    P.dma(posi[:r, :], pos_ap.partition_broadcast(r), [], [posiB])
    P.op("dve", lambda v: v.tensor_copy(posf[:r, :], posi[:r, :]), [posiB], [posfB])
    pidx, pidxB = P.sb("r_pidx", [128, 4], F32)
    P.op("pool", lambda g: g.iota(pidx[:, 0:1], pattern=[[0, 1]], base=0, channel_multiplier=1,
                                  allow_small_or_imprecise_dtypes=True), [], [pidxB])
    ts(P, pidx[:, 3:4], pidx[:, 0:1], float(half), None, ALU.is_ge, None, [pidxB], [pidxB])
    stt(P, pidx[:, 1:2], pidx[:, 3:4], -float(half), pidx[:, 0:1], ALU.mult, ALU.add, [pidxB], [pidxB])
    act(P, pidx[:, 2:3], pidx[:, 1:2], AF.Exp, [pidxB], [pidxB], scale=-float(np.log(THETA)) / half)
    ts(P, pidx[:, 3:4], pidx[:, 3:4], 2.0, -1.0, ALU.mult, ALU.add, [pidxB], [pidxB])
    TWO_PI = float(2 * np.pi)
    if nrow > r:
        P.op("dve", lambda v: v.memset(C2[:nrow, :], 1.0), [], [tB])
        P.op("dve", lambda v: v.memset(S2[:nrow, :], 0.0), [], [tB])
    ang, angB = P.sb("r_ang", [128, S], F32)
    ts(P, ang[:r, :], posf[:r, :], pidx[:r, 2:3], None, ALU.mult, None, [posfB, pidxB], [angB])
    wrap_pi(P, ang[:r, :], angB, posi[:r, :], posiB, posf[:r, :], posfB)
    act(P, S2[:r, :S], ang[:r, :], AF.Sin, [angB], [tB])
    ts(P, S2[:r, :S], S2[:r, :S], pidx[:r, 3:4], None, ALU.mult, None, [tB, pidxB], [tB])
    ts(P, ang[:r, :], ang[:r, :], float(np.pi / 2), None, ALU.add, None, [angB], [angB])
    ts(P, posf[:r, :], ang[:r, :], float(np.pi), None, ALU.is_gt, None, [angB], [posfB])
    stt(P, ang[:r, :], posf[:r, :], -TWO_PI, ang[:r, :], ALU.mult, ALU.add, [posfB, angB], [angB])
    act(P, C2[:r, :S], ang[:r, :], AF.Sin, [angB], [tB])
    P.pop()


class AttnBufs:
    def __init__(self, P, pp_acc):
        self.pT = [P.sb("at_pT%d" % i, [128, 512], BF16) for i in range(3)]
        self.i = 0
        self.rz, self.rzB = P.sb("at_rz", [128, 512], F32)
        self.acc = pp_acc

    def next_pT(self):
        t = self.pT[self.i % 3]
        self.i += 1
        return t


def attention_group(P, pp, K, ab, G, kcs, parts, vtok, vB, scale, out_ap, outB, mask_of, extra=None,
                    post=None):
    (po, poB), (pz, pzB) = ab.acc
    nk = len(kcs)
    for i, kc in enumerate(kcs):
        ps, psB = pp.get()
        ops = []
        for (kfn, q_ap, bufs) in parts:
            ops.append((kfn(kc), q_ap, bufs))
        m = mask_of(kc)
        if m is not None:
            ops.append((K.identb[:], K.mask[:, m, :], [K.identbB, K.maskB]))
        if extra is not None:
            ex = extra(kc)
            if ex is not None:
                ops.append(ex)
        for j, (l, r, bufs) in enumerate(ops):
            mm(P, ps[:], l, r, j == 0, j == len(ops) - 1, bufs, [psB])
        pT, pTB = ab.next_pT()
        act(P, pT[:], ps[:], AF.Exp, [psB], [pTB], scale=scale)
        mm(P, po[:], vtok(kc), pT[:], i == 0, i == nk - 1, [vB, pTB], [poB])
        mm(P, pz[:], K.onesb[:], pT[:], i == 0, i == nk - 1, [K.onesbB, pTB], [pzB])
    ts(P, ab.rz[:], pz[:], 1e-30, None, ALU.max, None, [pzB], [ab.rzB])
    P.op("dve", lambda v: v.reciprocal(ab.rz[:], ab.rz[:]), [ab.rzB], [ab.rzB])
    if post is None:
        tt(P, out_ap, po[:], ab.rz[:], ALU.mult, [poB, ab.rzB], [outB])
    else:
        post(po, poB, ab.rz, ab.rzB)


def norm_rows_to_bf16(P, pp, K, src_dram, nchunk, dim, gT, gB, dst, dstB, stg, stgB, sq, sqB, rb, rbB):
    sv = src_dram.rearrange("(c p) t -> p c t", p=128)
    for tl in range(S // 512):
        P.dma(stg[:, :nchunk, :], sv[:, :, tl * 512:(tl + 1) * 512], [], [stgB])
        act(P, sq[:, :nchunk, :], stg[:, :nchunk, :], AF.Square, [stgB], [sqB])
        ps, psB = pp.get()
        for c in range(nchunk):
            mm(P, ps[:], K.onesb[:], sq[:, c, :], c == 0, c == nchunk - 1, [K.onesbB, sqB], [psB])
        rstd_from_sumsq(P, rb, rbB, ps[:], psB, 512, dim)
        for c in range(nchunk):
            stt(P, dst[:, c, tl * 512:(tl + 1) * 512], stg[:, c, :], gT[:, c:c + 1], rb[:], ALU.mult, ALU.mult,
                [stgB, gB, rbB], [dstB])


def mixer_mla(P, pp, K, io):
    proj = io["proj"]
    pj = [io["projB"]]
    SCALE = 192 ** -0.5
    C2, tB = P.sb("m_C2", [64, S], F32)
    S2, _ = P.sb("m_S2", [64, S], F32)
    rope_tables(P, io["pos"][0], 64, 32, C2, S2, tB, S)
    gq, gqB = P.sb("m_gq", [128, 3], F32)
    gk, gkB = P.sb("m_gk", [128, 2], F32)
    P.dma(gq[:], io["qnT"], [], [gqB])
    P.dma(gk[:], io["kvnT"], [], [gkB])
    wq, wqB = P.sb("m_wq", [128, 3, 512], BF16)
    wkv, wkvB = P.sb("m_wkv", [128, 2, 512], BF16)
    P.dma(wq[:], io["wq"], [], [wqB], q="pool")
    P.dma(wkv[:], io["wkv"], [], [wkvB], q="pool")
    cqn, cqnB = P.sb("m_cqn", [128, 3, S], BF16)
    ckvn, ckvnB = P.sb("m_ckvn", [128, 2, S], BF16)
    stg, stgB = P.sb("m_stg", [128, 3, 512], F32)
    sq, sqB = P.sb("m_sq", [128, 3, 512], BF16)
    rb, rbB = P.sb("m_rb", [128, 512], F32)
    norm_rows_to_bf16(P, pp, K, proj[R_CQ:R_CQ + 384, :], 3, 384, gq, gqB, cqn, cqnB, stg, stgB, sq, sqB, rb, rbB)
    norm_rows_to_bf16(P, pp, K, proj[R_CKV:R_CKV + 256, :], 2, 256, gk, gkB, ckvn, ckvnB, stg, stgB, sq, sqB, rb, rbB)
    kpe, kpeB = P.sb("m_kpe", [64, 2, 512], F32)
    krot, krotB = P.sb("m_krot", [65, S], BF16)
    kn = [P.sb("m_kn%d" % h, [128, S], BF16) for h in range(2)]
    vt = [P.sb("m_vt%d" % h, [128, 32, 128], BF16) for h in range(2)]
    ksq, ksqB = P.sb("m_ksq", [128, 2, 512], BF16)
    kmax, kmaxB = P.sb("m_kmax", [128, 2, 8], F32)
    t1, t1B = P.sb("m_t1", [128, 512], F32)
    t2, t2B = P.sb("m_t2", [128, 512], F32)
    P.op("dve", lambda v: v.memset(krot[64:65, :], 1.0), [], [krotB])
    for tl in range(8):
        sl = slice(tl * 512, (tl + 1) * 512)
        P.dma(kpe[0:64, 0, :], proj[R_KPE:R_KPE + 64, sl], pj, [kpeB])
        P.dma(kpe[0:32, 1, :], proj[R_KPE + 32:R_KPE + 64, sl], pj, [kpeB])
        P.dma(kpe[32:64, 1, :], proj[R_KPE:R_KPE + 32, sl], pj, [kpeB])
        tt(P, t1[:64, :], kpe[:, 0, :], C2[:, sl], ALU.mult, [kpeB, tB], [t1B])
        tt(P, t2[:64, :], kpe[:, 1, :], S2[:, sl], ALU.mult, [kpeB, tB], [t2B])
        tt(P, krot[0:64, sl], t1[:64, :], t2[:64, :], ALU.add, [t1B, t2B], [krotB])
        act(P, ksq[0:64, 1, :], krot[0:64, sl], AF.Square, [krotB], [ksqB])
        for h in range(2):
            ps, psB = pp.get()
            for kc in range(2):
                mm(P, ps[:], wkv[:, kc, h * 256:h * 256 + 128], ckvn[:, kc, sl], kc == 0, kc == 1,
                   [wkvB, ckvnB], [psB])
            act(P, kn[h][0][:, sl], ps[:], AF.Copy, [psB], [kn[h][1]])
            act(P, ksq[:, 0, :], ps[:], AF.Square, [psB], [ksqB])
            ps2, ps2B = pp.get()
            mm(P, ps2[:], K.onesb[:], ksq[:, 0, :], True, False, [K.onesbB, ksqB], [ps2B])
            mm(P, ps2[:], K.onesb[0:64, :], ksq[0:64, 1, :], False, True, [K.onesbB, ksqB], [ps2B])
            P.op("dve", lambda v, ps2=ps2, h=h, tl=tl: v.reduce_max(kmax[:, h, tl:tl + 1], ps2[:], AX.X),
                 [ps2B], [kmaxB])
            for tk in range(4):
                ck = tl * 4 + tk
                ps3, ps3B = pp.get()
                for kc in range(2):
                    mm(P, ps3[:, 0:128], ckvn[:, kc, ck * 128:(ck + 1) * 128], wkv[:, kc, h * 256 + 128:h * 256 + 256],
                       kc == 0, kc == 1, [wkvB, ckvnB], [ps3B])
                P.op("dve", lambda v, ps3=ps3, h=h, ck=ck: v.tensor_copy(vt[h][0][:, ck, :], ps3[:, 0:128]),
                     [ps3B], [vt[h][1]])
    km, kmB = P.sb("m_km", [128, 2], F32)
    for h in range(2):
        P.op("dve", lambda v, h=h: v.reduce_max(km[:, h:h + 1], kmax[:, h, :], AX.X), [kmaxB], [kmB])
    qn, qnB = P.sb("m_qn", [128, S], BF16)
    qrot, qrotB = P.sb("m_qrot", [65, S], BF16)
    qsq, qsqB = P.sb("m_qsq", [128, 2, 512], BF16)
    ob, obB = P.sb("m_ob", [128, 512], F32)
    ab = AttnBufs(P, pp.acc)
    for h in range(2):
        for tl in range(8):
            sl = slice(tl * 512, (tl + 1) * 512)
            ps, psB = pp.get()
            for kc in range(3):
                mm(P, ps[:], wq[:, kc, h * 256:h * 256 + 128], cqn[:, kc, sl], kc == 0, kc == 2, [wqB, cqnB], [psB])
            act(P, qn[:, sl], ps[:], AF.Copy, [psB], [qnB])
            act(P, qsq[:, 0, :], ps[:], AF.Square, [psB], [qsqB])
            pr, prB = pp.get()
            for kc in range(3):
                mm(P, pr[0:64, :], wq[:, kc, h * 256 + 128:h * 256 + 192], cqn[:, kc, sl], kc == 0, kc == 2,
                   [wqB, cqnB], [prB])
            pw, pwB = pp.get()
            for kc in range(3):
                mm(P, pw[0:64, :], wq[:, kc, h * 256 + 192:h * 256 + 256], cqn[:, kc, sl], kc == 0, kc == 2,
                   [wqB, cqnB], [pwB])
            tt(P, t1[:64, :], pr[0:64, :], C2[:, sl], ALU.mult, [prB, tB], [t1B])
            tt(P, t2[:64, :], pw[0:64, :], S2[:, sl], ALU.mult, [pwB, tB], [t2B])
            tt(P, qrot[0:64, sl], t1[:64, :], t2[:64, :], ALU.add, [t1B, t2B], [qrotB])
            act(P, qsq[0:64, 1, :], qrot[0:64, sl], AF.Square, [qrotB], [qsqB])
            pq, pqB = pp.get()
            mm(P, pq[:], K.onesb[:], qsq[:, 0, :], True, False, [K.onesbB, qsqB], [pqB])
            mm(P, pq[:], K.onesb[0:64, :], qsq[0:64, 1, :], False, True, [K.onesbB, qsqB], [pqB])
            ts(P, t1[64:65, :], pq[64:65, :], km[64:65, h:h + 1], None, ALU.mult, None, [pqB, kmB], [t1B])
            act(P, t1[64:65, :], t1[64:65, :], AF.Sqrt, [t1B], [t1B])
            ts(P, qrot[64:65, sl], t1[64:65, :], -1.0, None, ALU.mult, None, [t1B], [qrotB])
        for G in range(8):
            kcs = list(range(4 * G + 4))
            parts = [(lambda kc, h=h: kn[h][0][:, kc * 128:(kc + 1) * 128], qn[:, G * 512:(G + 1) * 512], [kn[h][1], qnB]),
                     (lambda kc: krot[:, kc * 128:(kc + 1) * 128], qrot[:, G * 512:(G + 1) * 512], [krotB, qrotB])]
            attention_group(P, pp, K, ab, G, kcs, parts, lambda kc, h=h: vt[h][0][:, kc, :], vt[h][1], SCALE,
                            ob[:], obB, lambda kc, G=G: (kc - 4 * G) if kc >= 4 * G else None)
            P.dma(io["o_mla"][h * 128:(h + 1) * 128, G * 512:(G + 1) * 512], ob[:], [obB], [io["oB"]], is_output=True)


SL = 256


def cmul_bc(P, outr, outi, ar, ai, wr, wi, rd, wrB, t1, t1B, t2, t2B, e="dve", neg_i=False):
    tt(P, t1, ai, wi, ALU.mult, rd, [t1B], e=e)
    tt(P, t2, ar, wr, ALU.mult, rd, [t2B], e=e)
    tt(P, outr, t2, t1, ALU.subtract, [t1B, t2B], [wrB], e=e)
    tt(P, t1, ar, wi, ALU.mult, rd, [t1B], e=e)
    tt(P, t2, ai, wr, ALU.mult, rd, [t2B], e=e)
    if neg_i:
        tt(P, t1, t1, t2, ALU.add, [t1B, t2B], [t1B], e=e)
        ts(P, outi, t1, -1.0, None, ALU.mult, None, [t1B], [wrB], e=e)
    else:
        tt(P, outi, t1, t2, ALU.add, [t1B, t2B], [wrB], e=e)


def mixer_s5(P, pp, K, io):
    proj = io["proj"]
    pj = [io["projB"]]
    prm, prmB = P.sb("s_prm", [128, 3, 16], F32)
    P.dma(prm[:], io["s5p"], [], [prmB])
    c, cB = P.sb("s_c", [128, 16, 16], F32)
    STEP, MAG, TH, LR, LI, GR, GI, DEN, T1, T2, T3 = range(11)
    are, aim, lst = prm[:, 0, :], prm[:, 1, :], prm[:, 2, :]
    R = [prmB, cB]
    act(P, c[:, STEP, :], lst, AF.Exp, [prmB], [cB])
    tt(P, c[:, T1, :], are, c[:, STEP, :], ALU.mult, R, [cB])
    act(P, c[:, MAG, :], c[:, T1, :], AF.Exp, [cB], [cB])
    tt(P, c[:, TH, :], aim, c[:, STEP, :], ALU.mult, R, [cB])
    TWO_PI = float(2 * np.pi)
    ki, kiB = P.sb("s_ki", [128, 16], I32)
    wrap_pi(P, c[:, TH, :], cB, ki[:], kiB, c[:, T1, :], cB)
    act(P, c[:, T2, :], c[:, TH, :], AF.Sin, [cB], [cB])
    ts(P, c[:, 11, :], c[:, TH, :], float(np.pi / 2), None, ALU.add, None, [cB], [cB])
    ts(P, c[:, T1, :], c[:, 11, :], float(np.pi), None, ALU.is_gt, None, [cB], [cB])
    stt(P, c[:, 11, :], c[:, T1, :], -TWO_PI, c[:, 11, :], ALU.mult, ALU.add, [cB], [cB])
    act(P, c[:, T3, :], c[:, 11, :], AF.Sin, [cB], [cB])
    P.op("dve", lambda v: v.tensor_copy(c[:, 12, :], c[:, T2, :]), [cB], [cB])
    P.op("dve", lambda v: v.tensor_copy(c[:, 13, :], c[:, T3, :]), [cB], [cB])
    tt(P, c[:, LR, :], c[:, MAG, :], c[:, T3, :], ALU.mult, [cB], [cB])
    tt(P, c[:, LI, :], c[:, MAG, :], c[:, T2, :], ALU.mult, [cB], [cB])
    tt(P, c[:, DEN, :], are, are, ALU.mult, R, [cB])
    tt(P, c[:, T1, :], aim, aim, ALU.mult, R, [cB])
    tt(P, c[:, DEN, :], c[:, DEN, :], c[:, T1, :], ALU.add, [cB], [cB])
    P.op("dve", lambda v: v.reciprocal(c[:, DEN, :], c[:, DEN, :]), [cB], [cB])
    ts(P, c[:, T1, :], c[:, LR, :], -1.0, None, ALU.add, None, [cB], [cB])
    tt(P, c[:, T2, :], c[:, T1, :], are, ALU.mult, R, [cB])
    tt(P, c[:, T3, :], c[:, LI, :], aim, ALU.mult, R, [cB])
    tt(P, c[:, GR, :], c[:, T2, :], c[:, T3, :], ALU.add, [cB], [cB])
    tt(P, c[:, GR, :], c[:, GR, :], c[:, DEN, :], ALU.mult, [cB], [cB])
    tt(P, c[:, T2, :], c[:, LI, :], are, ALU.mult, R, [cB])
    tt(P, c[:, T3, :], c[:, T1, :], aim, ALU.mult, R, [cB])
    tt(P, c[:, GI, :], c[:, T2, :], c[:, T3, :], ALU.subtract, [cB], [cB])
    tt(P, c[:, GI, :], c[:, GI, :], c[:, DEN, :], ALU.mult, [cB], [cB])
    w, wB = P.sb("s_w", [128, 10, 2, 16], F32)
    P.op("dve", lambda v: v.tensor_copy(w[:, 0, 0, :], c[:, 13, :]), [cB], [wB])
    P.op("dve", lambda v: v.tensor_copy(w[:, 0, 1, :], c[:, 12, :]), [cB], [wB])
    sA, sAB = P.sb("s_sA", [128, 16, SL // 2], F32)
    sB_, sBB = P.sb("s_sB", [128, 16, SL // 2], F32)
    nlev = int(np.log2(SL))
    for k in range(nlev):
        cmul_bc(P, w[:, k + 1, 0, :], w[:, k + 1, 1, :], w[:, k, 0, :], w[:, k, 1, :], w[:, k, 0, :], w[:, k, 1, :],
                [wB], wB, sA[:, :, 0], sAB, sB_[:, :, 0], sBB)
    Ep, EpB = P.sb("s_Ep", [128, 2, 16, SL], F32)
    Em, EmB = P.sb("s_Em", [128, 2, 16, SL], F32)
    P.op("dve", lambda v: v.memset(Ep[:, 0, :, 0:1], 1.0), [], [EpB])
    P.op("dve", lambda v: v.memset(Ep[:, 1, :, 0:1], 0.0), [], [EpB])
    for k in range(nlev):
        n = 1 << k
        wr = w[:, k, 0, :].unsqueeze(2).to_broadcast([128, 16, n])
        wi = w[:, k, 1, :].unsqueeze(2).to_broadcast([128, 16, n])
        cmul_bc(P, Ep[:, 0, :, n:2 * n], Ep[:, 1, :, n:2 * n], Ep[:, 0, :, 0:n], Ep[:, 1, :, 0:n], wr, wi,
                [EpB, wB], EpB, sA[:, :, 0:n], sAB, sB_[:, :, 0:n], sBB)
    for half in range(2):
        hs = slice(half * SL // 2, (half + 1) * SL // 2)
        gr = c[:, GR, :].unsqueeze(2).to_broadcast([128, 16, SL // 2])
        gi = c[:, GI, :].unsqueeze(2).to_broadcast([128, 16, SL // 2])
        tt(P, sA[:], Ep[:, 0, :, hs], gr, ALU.mult, [EpB, cB], [sAB])
        tt(P, sB_[:], Ep[:, 1, :, hs], gi, ALU.mult, [EpB, cB], [sBB])
        tt(P, Em[:, 0, :, hs], sA[:], sB_[:], ALU.add, [sAB, sBB], [EmB])
        tt(P, sA[:], Ep[:, 0, :, hs], gi, ALU.mult, [EpB, cB], [sAB])
        tt(P, sB_[:], Ep[:, 1, :, hs], gr, ALU.mult, [EpB, cB], [sBB])
        tt(P, Em[:, 1, :, hs], sA[:], sB_[:], ALU.subtract, [sAB, sBB], [EmB])
    Bm, BmB = P.sb("s_B", [128, 2, 16, 128], BF16)
    Cm, CmB = P.sb("s_C", [128, 2, 16, 128], BF16)
    P.dma(Bm[:, 0], io["Bre"], [], [BmB], q="pool")
    P.dma(Bm[:, 1], io["Bim"], [], [BmB], q="pool")
    P.dma(Cm[:, 0], io["Cre"], [], [CmB], q="pool")
    P.dma(Cm[:, 1], io["Cim"], [], [CmB], q="pool")
    wg, wgB = P.sb("s_wg", [128, 4, 256], BF16)
    P.dma(wg[:], io["wglu"], [], [wgB], q="pool")
    sm, smB = P.sb("s_sm", [128, 8], F32)
    P.dma(sm[:, 0:4], io["dT"], [], [smB])
    P.dma(sm[:, 4:6], io["bgT"], [], [smB])
    uf, ufB = P.sb("s_uf", [128, 4, SL], F32)
    ub, ubB = P.sb("s_ub", [128, 4, SL], BF16)
    bp, bpB = P.sb("s_bp", [128, 2, 2, SL], F32)
    ws, wsB = P.sb("s_ws", [128, 2, 16, SL], F32)
    xb, xbB = P.sb("s_xb", [128, 2, 16, SL], BF16)
    init, initB = P.sb("s_init", [128, 2, 16], F32)
    ta, taB = P.sb("s_ta", [128, 2, SL], F32)
    tb, tbB = P.sb("s_tb", [128, 2, SL], F32)
    tc_, tcB = P.sb("s_tc", [128, 8, SL], F32)
    td, tdB = P.sb("s_td", [128, 8, SL], F32)
    y2, y2B = P.sb("s_y2", [128, 4, SL], BF16)
    yt, ytB = P.sb("s_yt", [128, SL], F32)
    ob, obB = P.sb("s_ob", [128, 2, SL], F32)
    uv = proj[R_U:R_U + 512, :].rearrange("(c p) t -> p c t", p=128)
    P.op("dve", lambda v: v.memset(init[:], 0.0), [], [initB])
    for tl in range(S // SL):
        sl = slice(tl * SL, (tl + 1) * SL)
        P.dma(uf[:], uv[:, :, sl], pj, [ufB])
        act(P, ub[:], uf[:], AF.Copy, [ufB], [ubB])
        if tl > 0:
            cmul_bc(P, init[:, 0, :], init[:, 1, :], ws[:, 0, :, SL - 1], ws[:, 1, :, SL - 1],
                    w[:, nlev, 0, :], w[:, nlev, 1, :], [wsB, wB], initB, sA[:, :, 0], sAB, sB_[:, :, 0], sBB)
        for pr in range(8):
            pbr, pbrB = pp.get()
            pbi, pbiB = pp.get()
            for q in range(2):
                sc = 2 * pr + q
                mm(P, pbr[:, q * SL:(q + 1) * SL], Bm[:, 0, sc, :], ub[:, sc // 4, :], True, True, [BmB, ubB], [pbrB])
                mm(P, pbi[:, q * SL:(q + 1) * SL], Bm[:, 1, sc, :], ub[:, sc // 4, :], True, True, [BmB, ubB], [pbiB])
            br = pbr[:].rearrange("p (q t) -> p q t", q=2)
            bi = pbi[:].rearrange("p (q t) -> p q t", q=2)
            scs = slice(2 * pr, 2 * pr + 2)
            tt(P, ta[:], br, Em[:, 0, scs, :], ALU.mult, [pbrB, EmB], [taB])
            tt(P, tb[:], bi, Em[:, 1, scs, :], ALU.mult, [pbiB, EmB], [tbB])
            tt(P, bp[:, 0], ta[:], tb[:], ALU.subtract, [taB, tbB], [bpB])
            tt(P, ta[:], br, Em[:, 1, scs, :], ALU.mult, [pbrB, EmB], [taB])
            tt(P, tb[:], bi, Em[:, 0, scs, :], ALU.mult, [pbiB, EmB], [tbB])
            tt(P, bp[:, 1], ta[:], tb[:], ALU.add, [taB, tbB], [bpB])
            for q in range(2):
                sc = 2 * pr + q
                for ri in range(2):
                    P.op("dve", lambda v, sc=sc, ri=ri, q=q: v.tensor_tensor_scan(
                        ws[:, ri, sc, :], c[:, MAG, sc:sc + 1].to_broadcast([128, SL]), bp[:, ri, q, :],
                        init[:, ri, sc:sc + 1], ALU.mult, ALU.add),
                        [cB, bpB, initB], [wsB])
        for hh_ in range(2):
            h8 = slice(hh_ * 8, hh_ * 8 + 8)
            cmul_bc(P, xb[:, 0, h8], xb[:, 1, h8], ws[:, 0, h8], ws[:, 1, h8], Ep[:, 0, h8], Ep[:, 1, h8],
                    [wsB, EpB], xbB, tc_[:], tcB, td[:], tdB, e="pool", neg_i=True)
        for uc in range(4):
            py, pyB = pp.get()
            for q in range(4):
                sc = 4 * uc + q
                mm(P, py[:, :SL], Cm[:, 0, sc, :], xb[:, 0, sc, :], q == 0, False, [CmB, xbB], [pyB])
                mm(P, py[:, :SL], Cm[:, 1, sc, :], xb[:, 1, sc, :], False, q == 3, [CmB, xbB], [pyB])
            stt(P, yt[:], uf[:, uc, :], sm[:, uc:uc + 1], py[:, :SL], ALU.mult, ALU.add, [ufB, smB, pyB], [ytB])
            act(P, y2[:, uc, :], yt[:], AF.Gelu_apprx_tanh, [ytB], [y2B])
        for oc in range(2):
            pg, pgB = pp.get()
            for kc in range(4):
                mm(P, pg[:, :SL], wg[:, kc, oc * 128:(oc + 1) * 128], y2[:, kc, :], kc == 0, kc == 3, [wgB, y2B], [pgB])
            act(P, yt[:], pg[:, :SL], AF.Sigmoid, [pgB, smB], [ytB], bias=sm[:, 4 + oc:5 + oc], scale=1.0)
            tt(P, ob[:, oc, :], y2[:, oc, :], yt[:], ALU.mult, [y2B, ytB], [obB])
        P.dma(io["o_s5"][:, sl].rearrange("(c p) t -> p c t", p=128), ob[:], [obB], [io["oB"]], is_output=True)


def host_mla_inputs(inp, l, b, hf):
    wuq = inp["mla_w_uq"][l]
    wukv = inp["mla_w_ukv"][l]
    qcols = []
    for hh in range(2):
        h = 2 * hf + hh
        base = h * 192
        qcols += list(range(base, base + 128))
        qcols += list(range(base + 128, base + 192))
        qcols += list(range(base + 160, base + 192)) + list(range(base + 128, base + 160))
    kvcols = []
    for hh in range(2):
        h = 2 * hf + hh
        kvcols += list(range(h * 256, h * 256 + 256))
    return {
        "pos": np.ascontiguousarray(inp["positions"][b][None, :]).astype(np.int32),
        "qnT": vecT(inp["mla_q_norm"][l]), "kvnT": vecT(inp["mla_kv_norm"][l]),
        "wq": np.ascontiguousarray(wuq[:, qcols].reshape(3, 128, 512).transpose(1, 0, 2)),
        "wkv": np.ascontiguousarray(wukv[:, kvcols].reshape(2, 128, 512).transpose(1, 0, 2)),
    }


def s5_perm(hf):
    return [2 * hf, 2 * hf + 1, 2 * (1 - hf), 2 * (1 - hf) + 1]


def host_s5_inputs(inp, l, hf):
    perm = s5_perm(hf)
    gperm = np.concatenate([np.arange(8 * c, 8 * c + 8) for c in perm])
    chperm = np.concatenate([np.arange(128 * c, 128 * c + 128) for c in perm])

    def st(v):
        return np.ascontiguousarray(v[gperm].reshape(16, 128).T)

    lst = np.repeat(inp["s5_log_step"][l][:, None], 64, axis=1)
    s5p = np.stack([st(inp["s5_a_re"][l]), st(inp["s5_a_im"][l]), st(lst)], axis=1)

    def bmat(Bv):
        Bv = Bv[gperm]
        out = np.zeros((128, 16, 128), np.float32)
        for sc in range(16):
            for gl in range(2):
                g = 2 * sc + gl
                r0 = (sc % 4) * 32 + gl * 16
                out[r0:r0 + 16, sc, gl * 64:(gl + 1) * 64] = Bv[g].T
        return out

    def cmat(Cv):
        Cv = Cv[gperm]
        out = np.zeros((128, 16, 128), np.float32)
        for sc in range(16):
            for gl in range(2):
                g = 2 * sc + gl
                c0 = (sc % 4) * 32 + gl * 16
                out[gl * 64:(gl + 1) * 64, sc, c0:c0 + 16] = Cv[g].T
        return out

    wglu = inp["s5_w_glu"][l][chperm][:, chperm[:256]]
    return {
        "s5p": np.ascontiguousarray(s5p.astype(np.float32)),
        "Bre": bmat(inp["s5_b_re"][l]), "Bim": bmat(inp["s5_b_im"][l]),
        "Cre": cmat(inp["s5_c_re"][l]), "Cim": cmat(inp["s5_c_im"][l]),
        "dT": vecT(inp["s5_d"][l][chperm]),
        "wglu": np.ascontiguousarray(wglu.reshape(4, 128, 256).transpose(1, 0, 2)),
        "bgT": vecT(inp["s5_b_glu"][l][chperm[:256]]),
    }


def host_gdn_inputs(inp, l, hf):
    cw = inp["gdn_conv_w"][l]
    cwT = np.zeros((128, 3, 2, 4), np.float32)
    for x in range(3):
        for hh in range(2):
            c0 = x * 512 + (2 * hf + hh) * 128
            cwT[:, x, hh, :] = cw[:, c0:c0 + 128].T
    st = lambda v: np.ascontiguousarray(np.repeat(v[2 * hf:2 * hf + 2], 64)[:, None].astype(np.float32))
    return {"cwT": cwT, "alog": st(inp["gdn_a_log"][l]), "dtb": st(inp["gdn_dt_bias"][l]),
            "onT": np.ascontiguousarray(inp["gdn_o_norm"][l][:, None])}


def mixer_gdn(P, pp, K, io):
    proj = io["proj"]
    pj = [io["projB"]]
    NCK = S // 64
    sm, smB = P.sb("g_sm", [128, 8], F32)
    P.dma(sm[:, 0:1], io["alog"], [], [smB])
    P.dma(sm[:, 1:2], io["dtb"], [], [smB])
    P.dma(sm[:, 2:3], io["onT"], [], [smB])
    act(P, sm[:, 3:4], sm[:, 0:1], AF.Exp, [smB], [smB])
    ts(P, sm[:, 3:4], sm[:, 3:4], -1.0, None, ALU.mult, None, [smB], [smB])
    cw, cwB = P.sb("g_cw", [128, 3, 2, 4], F32)
    P.dma(cw[:], io["cwT"], [], [cwB])
    ubd, ubdB = P.sb("g_ubd", [128, 128], F32)
    usd, usdB = P.sb("g_usd", [128, 128], F32)
    madd, maddB = P.sb("g_madd", [128, 128], F32)
    bones, bonesB = P.sb("g_bones", [128, 128], F32)
    selh, selhB = P.sb("g_selh", [128, 2, 128], F32)
    ts(P, ubd[:], K.d0[:, 0:128], 0.0, None, ALU.is_ge, None, [K.d0B], [ubdB])
    P.op("dve", lambda v: v.memset(ubd[0:64, 64:128], 0.0), [], [ubdB])
    ts(P, usd[:], K.d0[:, 0:128], 0.0, None, ALU.is_gt, None, [K.d0B], [usdB])
    P.op("dve", lambda v: v.memset(usd[0:64, 64:128], 0.0), [], [usdB])
    ts(P, madd[:], ubd[:], 1.0, -NEG, ALU.subtract, ALU.mult, [ubdB], [maddB])
    P.op("dve", lambda v: v.memset(bones[:], 0.0), [], [bonesB])
    P.op("dve", lambda v: v.memset(bones[0:64, 0:64], 1.0), [], [bonesB])
    P.op("dve", lambda v: v.memset(bones[64:128, 64:128], 1.0), [], [bonesB])
    P.op("dve", lambda v: v.memset(selh[:], 0.0), [], [selhB])
    P.op("dve", lambda v: v.memset(selh[0:64, 0, :], 1.0), [], [selhB])
    P.op("dve", lambda v: v.memset(selh[64:128, 1, :], 1.0), [], [selhB])
    gst, gstB = P.sb("g_gst", [128, 8, NCK], F32)
    gab, gabB = P.sb("g_gab", [128, 2, 128], F32)
    P.op("dve", lambda v: v.memset(gab[:], 0.0), [], [gabB])
    for h in range(2):
        P.dma(gab[0:64, 0, 64 * h:64 * h + 64], proj[R_GA + h, :].rearrange("(c s) -> c s", s=64), pj, [gabB])
        P.dma(gab[0:64, 1, 64 * h:64 * h + 64], proj[R_GB + h, :].rearrange("(c s) -> c s", s=64), pj, [gabB])
    for i_ in range(2):
        ps, psB = pp.get()
        P.op("pe", lambda t, ps=ps, i_=i_: t.transpose(ps[:, 0:128], gab[:, i_, :], K.identf[:]),
             [gabB, K.identfB], [psB])
        P.op("dve", lambda v, ps=ps, i_=i_: v.tensor_copy(gst[:, i_, :], ps[:, 0:64]), [psB], [gstB])
    act(P, gst[:, 0, :], gst[:, 0, :], AF.Exp, [gstB, smB], [gstB], bias=sm[:, 1:2], scale=1.0)
    act(P, gst[:, 0, :], gst[:, 0, :], AF.Ln, [gstB], [gstB], bias=1.0, scale=1.0)
    ts(P, gst[:, 0, :], gst[:, 0, :], sm[:, 3:4], None, ALU.mult, None, [gstB, smB], [gstB])
    act(P, gst[:, 1, :], gst[:, 1, :], AF.Sigmoid, [gstB], [gstB])
    ps, psB = pp.get()
    mm(P, ps[:, 0:NCK], ubd[:], gst[:, 0, :], True, True, [ubdB, gstB], [psB])
    P.op("dve", lambda v: v.tensor_copy(gst[:, 2, :], ps[:, 0:NCK]), [psB], [gstB])
    act(P, gst[:, 3, :], gst[:, 2, :], AF.Exp, [gstB], [gstB])
    ps, psB = pp.get()
    mm(P, ps[:, 0:NCK], bones[:], gst[:, 0, :], True, True, [bonesB, gstB], [psB])
    P.op("dve", lambda v: v.tensor_copy(gst[:, 5, :], ps[:, 0:NCK]), [psB], [gstB])
    tt(P, gst[:, 4, :], gst[:, 5, :], gst[:, 2, :], ALU.subtract, [gstB], [gstB])
    act(P, gst[:, 4, :], gst[:, 4, :], AF.Exp, [gstB], [gstB])
    ts(P, gst[:, 6, :], gst[:, 4, :], selh[:, 0, 0:1], None, ALU.mult, None, [gstB, selhB], [gstB])
    ts(P, gst[:, 7, :], gst[:, 4, :], selh[:, 1, 0:1], None, ALU.mult, None, [gstB, selhB], [gstB])
    import os
    GSTOP = os.environ.get("GDN_STOP", "")
    GSUB = os.environ.get("GDN_SUB", "")
    egl, eglB = P.sb("g_egl", [128, 2, NCK], F32)
    for h in range(2):
        ps, psB = pp.get()
        mm(P, ps[:, 0:NCK], selh[:, h, :], gst[:, 0, :], True, True, [selhB, gstB], [psB])
        act(P, egl[:, h, :], ps[:, 0:NCK], AF.Exp, [psB], [eglB])
    if GSTOP == "0":
        P.dma(io["o_gdn"][0:128, 0:64], gst[:, 2, :], [gstB], [], is_output=True)
        return
    QT, QTB = P.sb("g_QT", [128, NCK, 2, 64], F32)
    KT, KTB = P.sb("g_KT", [128, NCK, 2, 64], F32)
    VT, VTB = P.sb("g_VT", [128, NCK, 2, 64], F32)
    GZ, GZB = P.sb("g_GZ", [128, NCK, 2, 64], F32)
    P.push()
    xp, xpB = P.sb("g_xp", [128, S + 3], F32)
    yc, ycB = P.sb("g_yc", [128, S], F32)
    sq, sqB = P.sb("g_sq", [128, 512], F32)
    rb, rbB = P.sb("g_rb", [128, 512], F32)
    P.op("dve", lambda v: v.memset(xp[:, 0:3], 0.0), [], [xpB])
    for xi, (row, dst, dstB) in enumerate([(R_GQ, QT, QTB), (R_GK, KT, KTB), (R_GV, VT, VTB)]):
        for h in range(2):
            P.dma(xp[:, 3:S + 3], proj[row + 128 * h:row + 128 * h + 128, :], pj, [xpB])
            ts(P, yc[:], xp[:, 0:S], cw[:, xi, h, 0:1], None, ALU.mult, None, [xpB, cwB], [ycB])
            for j in range(1, 4):
                stt(P, yc[:], xp[:, j:S + j], cw[:, xi, h, j:j + 1], yc[:], ALU.mult, ALU.add, [xpB, cwB, ycB], [ycB])
            act(P, yc[:], yc[:], AF.Silu, [ycB], [ycB])
            dview = dst[:, :, h, :]
            if xi == 2:
                P.op("dve", lambda v, dview=dview: v.tensor_copy(dview, yc[:].rearrange("p (c s) -> p c s", s=64)),
                     [ycB], [dstB])
            else:
                for tl in range(8):
                    sl = slice(tl * 512, (tl + 1) * 512)
                    act(P, sq[:], yc[:, sl], AF.Square, [ycB], [sqB])
                    ps, psB = pp.get()
                    mm(P, ps[:], K.onesf[:], sq[:], True, True, [K.onesfB, sqB], [psB])
                    ts(P, rb[:], ps[:], 1e-6, None, ALU.add, None, [psB], [rbB])
                    rsqrt_(P, rb[:], rbB)
                    if xi == 0:
                        ts(P, rb[:], rb[:], float(128 ** -0.5), None, ALU.mult, None, [rbB], [rbB])
                    tt(P, dst[:, tl * 8:(tl + 1) * 8, h, :], yc[:, sl].rearrange("p (c s) -> p c s", s=64),
                       rb[:].rearrange("p (c s) -> p c s", s=64), ALU.mult, [ycB, rbB], [dstB])
    for h in range(2):
        P.dma(xp[:, 3:S + 3], proj[R_GZ + 128 * h:R_GZ + 128 * h + 128, :], pj, [xpB])
        act(P, GZ[:, :, h, :], xp[:, 3:S + 3].rearrange("p (c s) -> p c s", s=64), AF.Silu, [xpB], [GZB])
    P.pop()
    if GSTOP == "1":
        P.dma(io["o_gdn"][0:128, 0:64], KT[:, 0, 0, :], [KTB], [], is_output=True)
        return
    def sq128(name):
        return P.sb(name, [128, 128], F32)
    rg, rgB = sq128("g_rg")
    dm, dmB = sq128("g_dm")
    DTi, DTiB = sq128("g_DTi")
    DTs, DTsB = sq128("g_DTs")
    Y = [sq128("g_Y%d" % i) for i in range(2)]
    Yt = [sq128("g_Yt%d" % i) for i in range(2)]
    Pm = [sq128("g_Pm%d" % i) for i in range(2)]
    Kst, KstB = sq128("g_Kst")
    Ke, KeB = sq128("g_Ke")
    Kd0, Kd0B = sq128("g_Kd0")
    Kd1, Kd1B = sq128("g_Kd1")
    Kdh = [(Kd0, Kd0B), (Kd1, Kd1B)]
    Vst, VstB = sq128("g_Vst")
    Ust, UstB = sq128("g_Ust")
    Wst, WstB = sq128("g_Wst")
    WT0, WT0B = sq128("g_WT0")
    WT1, WT1B = sq128("g_WT1")
    QT0, QT0B = sq128("g_QT0")
    QT1, QT1B = sq128("g_QT1")
    AT, ATB = sq128("g_AT")
    Vn, VnB = sq128("g_Vn")
    T1s, T1sB = sq128("g_T1s")
    Ost, OstB = sq128("g_Ost")
    junk, junkB = sq128("g_junk")
    St = [sq128("g_S%d" % h) for h in range(2)]
    ssq, ssqB = P.sb("g_ssq", [128, 2], F32)
    ob, obB = P.sb("g_ob", [128, 2, 8, 64], F32)
    for t_, b_ in (St[0], St[1], (WT0, WT0B), (WT1, WT1B), (QT0, QT0B), (QT1, QT1B)):
        P.op("dve", lambda v, t_=t_: v.memset(t_[:], 0.0), [], [b_])
    beta = gst[:, 1, :]
    for c in range(NCK if GSTOP != "2" else 8):
        kT = KT[:, c].rearrange("p h s -> p (h s)")
        qT = QT[:, c].rearrange("p h s -> p (h s)")
        vT = VT[:, c].rearrange("p h s -> p (h s)")
        cc = slice(c, c + 1)
        ts(P, rg[:], ubd[:], gst[:, 0, cc], None, ALU.mult, None, [ubdB, gstB], [rgB])
        ps, psB = pp.get()
        mm(P, ps[:, 0:128], K.onesf[:], rg[:], True, True, [K.onesfB, rgB], [psB])
        stt(P, dm[:], ps[:, 0:128], gst[:, 2, cc], madd[:], ALU.subtract, ALU.add, [psB, gstB, maddB], [dmB])
        act(P, DTi[:], dm[:], AF.Exp, [dmB], [DTiB])
        tt(P, DTs[:], DTi[:], usd[:], ALU.mult, [DTiB, usdB], [DTsB])
        if GSUB == "a":
            continue
        ps, psB = pp.get()
        mm(P, ps[:, 0:128], kT, kT, True, True, [KTB], [psB])
        stt(P, Y[0][0][:], ps[:, 0:128], beta[:, cc], DTs[:], ALU.mult, ALU.mult, [psB, gstB, DTsB], [Y[0][1]])
        ps, psB = pp.get()
        P.op("pe", lambda t, ps=ps: t.transpose(ps[:, 0:128], Y[0][0][:], K.identf[:]), [Y[0][1], K.identfB], [psB])
        act(P, Yt[0][0][:], ps[:, 0:128], AF.Copy, [psB], [Yt[0][1]])
        tt(P, Pm[0][0][:], K.identf[:], Y[0][0][:], ALU.subtract, [K.identfB, Y[0][1]], [Pm[0][1]])
        if GSUB == "c":
            continue
        cur = 0
        for k in range(5):
            nxt = 1 - cur
            psYt, psYtB = pp.get()
            mm(P, psYt[:, 0:128], Y[cur][0][:], Yt[cur][0][:], True, True, [Y[cur][1], Yt[cur][1]], [psYtB])
            if k < 4:
                psY, psYB = pp.get()
                mm(P, psY[:, 0:128], Yt[cur][0][:], Y[cur][0][:], True, True, [Y[cur][1], Yt[cur][1]], [psYB])
                act(P, Y[nxt][0][:], psY[:, 0:128], AF.Copy, [psYB], [Y[nxt][1]])
            P.op("dve", lambda v, nxt=nxt, psYt=psYt: v.tensor_copy(Yt[nxt][0][:], psYt[:, 0:128]), [psYtB], [Yt[nxt][1]])
            psP, psPB = pp.get()
            mm(P, psP[:, 0:128], Yt[nxt][0][:], Pm[cur][0][:], True, True, [Yt[nxt][1], Pm[cur][1]], [psPB])
            tt(P, Pm[nxt][0][:], Pm[cur][0][:], psP[:, 0:128], ALU.add, [Pm[cur][1], psPB], [Pm[nxt][1]])
            cur = nxt
        RT, RTB = Pm[cur]
        if GSUB == "d":
            continue
        ps, psB = pp.get()
        P.op("pe", lambda t, ps=ps, kT=kT: t.transpose(ps[:, 0:128], kT, K.identf[:]), [KTB, K.identfB], [psB])
        ts(P, Ke[:], ps[:, 0:128], gst[:, 3, cc], None, ALU.mult, None, [psB, gstB], [KeB])
        ts(P, Kd0[:], ps[:, 0:128], gst[:, 6, cc], None, ALU.mult, None, [psB, gstB], [Kd0B])
        ts(P, Kd1[:], ps[:, 0:128], gst[:, 7, cc], None, ALU.mult, None, [psB, gstB], [Kd1B])
        ps, psB = pp.get()
        P.op("pe", lambda t, ps=ps, vT=vT: t.transpose(ps[:, 0:128], vT, K.identf[:]), [VTB, K.identfB], [psB])
        act(P, Vst[:], ps[:, 0:128], AF.Copy, [psB], [VstB])
        if GSUB == "e1":
            continue
        ps, psB = pp.get()
        mm(P, ps[:, 0:128], RT[:], Vst[:], True, True, [RTB, VstB], [psB])
        ts(P, Ust[:], ps[:, 0:128], beta[:, cc], None, ALU.mult, None, [psB, gstB], [UstB])
        ps, psB = pp.get()
        mm(P, ps[:, 0:128], RT[:], Ke[:], True, True, [RTB, KeB], [psB])
        ts(P, Wst[:], ps[:, 0:128], beta[:, cc], None, ALU.mult, None, [psB, gstB], [WstB])
        if GSUB == "e2":
            continue
        ps, psB = pp.get()
        P.op("pe", lambda t, ps=ps: t.transpose(ps[:, 0:128], Wst[:], K.identf[:]), [WstB, K.identfB], [psB])
        P.op("dve", lambda v, ps=ps: v.tensor_copy(WT0[:, 0:64], ps[:, 0:64]), [psB], [WT0B])
        act(P, WT1[:, 64:128], ps[:, 64:128], AF.Copy, [psB], [WT1B])
        if GSUB == "e3":
            continue
        P.op("pool", lambda g, qT=qT: g.tensor_copy(QT0[:, 0:64], qT[:, 0:64]), [QTB], [QT0B])
        P.op("pool", lambda g, qT=qT: g.tensor_copy(QT1[:, 64:128], qT[:, 64:128]), [QTB], [QT1B])
        if GSUB == "e":
            continue
        ps, psB = pp.get()
        mm(P, ps[:, 0:128], kT, qT, True, True, [KTB, QTB], [psB])
        tt(P, AT[:], ps[:, 0:128], DTi[:], ALU.mult, [psB, DTiB], [ATB])
        if GSUB == "f":
            continue
        ps, psB = pp.get()
        mm(P, ps[:, 0:128], WT0[:], St[0][0][:], True, False, [WT0B, St[0][1]], [psB])
        mm(P, ps[:, 0:128], WT1[:], St[1][0][:], False, True, [WT1B, St[1][1]], [psB])
        tt(P, Vn[:], Ust[:], ps[:, 0:128], ALU.subtract, [UstB, psB], [VnB])
        ps, psB = pp.get()
        mm(P, ps[:, 0:128], QT0[:], St[0][0][:], True, False, [QT0B, St[0][1]], [psB])
        mm(P, ps[:, 0:128], QT1[:], St[1][0][:], False, True, [QT1B, St[1][1]], [psB])
        ts(P, T1s[:], ps[:, 0:128], gst[:, 3, cc], None, ALU.mult, None, [psB, gstB], [T1sB])
        ps, psB = pp.get()
        mm(P, ps[:, 0:128], AT[:], Vn[:], True, True, [ATB, VnB], [psB])
        tt(P, Ost[:], ps[:, 0:128], T1s[:], ALU.add, [psB, T1sB], [OstB])
        for h in range(2):
            hs = slice(64 * h, 64 * h + 64)
            ps, psB = pp.get()
            mm(P, ps[:, 0:128], Kdh[h][0][:], Vn[:], True, True, [Kdh[h][1], VnB], [psB])
            stt(P, St[h][0][:], St[h][0][:], egl[:, h, cc], ps[:, 0:128], ALU.mult, ALU.add,
                [St[h][1], eglB, psB], [St[h][1]])
        if GSUB == "g":
            continue
        act(P, junk[:], Ost[:], AF.Square, [OstB], [junkB])
        P.op("dve", lambda v: v.reduce_sum(ssq[:, 0:1], junk[:], AX.X), [junkB], [ssqB])
        ts(P, ssq[:, 1:2], ssq[:, 0:1], 1.0 / 128, EPS, ALU.mult, ALU.add, [ssqB], [ssqB])
        rsqrt_(P, ssq[:, 1:2], ssqB)
        ts(P, Ost[:], Ost[:], ssq[:, 1:2], None, ALU.mult, None, [OstB, ssqB], [OstB])
        ps, psB = pp.get()
        P.op("pe", lambda t, ps=ps: t.transpose(ps[:, 0:128], Ost[:], K.identf[:]), [OstB, K.identfB], [psB])
        stt(P, ob[:, :, c % 8, :], ps[:, 0:128].rearrange("p (h s) -> p h s", h=2), sm[:, 2:3], GZ[:, c], ALU.mult, ALU.mult,
            [psB, smB, GZB], [obB])
        if c % 8 == 7:
            c0 = (c - 7) * 64
            P.dma(io["o_gdn"][:, c0:c0 + 512].rearrange("(h p) t -> p h t", p=128),
                  ob[:].rearrange("p h c s -> p h (c s)"), [obB], [io["oB"]], is_output=True)


def host_nsa_inputs(inp, l, b, hf):
    def w1(v):
        return np.ascontiguousarray(v.reshape(32, 128, 128).transpose(1, 0, 2))
    return {
        "pos": np.ascontiguousarray(inp["positions"][b][None, :]).astype(np.int32),
        "w1k": w1(inp["nsa_w1_k"][l]), "w1v": w1(inp["nsa_w1_v"][l]),
        "w2k": np.ascontiguousarray(inp["nsa_w2_k"][l]), "w2v": np.ascontiguousarray(inp["nsa_w2_v"][l]),
        "poskT": np.ascontiguousarray(inp["nsa_pos_k"][l].T), "posvT": np.ascontiguousarray(inp["nsa_pos_v"][l].T),
    }


def mixer_nsa(P, pp, K, io):
    proj = io["proj"]
    pj = [io["projB"]]
    SCALE = 128 ** -0.5
    SP = S + 32
    qr = [P.sb("n_qr%d" % h, [128, S], BF16) for h in range(4)]
    kcr, kcrB = P.sb("n_kcr", [128, SP], BF16)
    vcb, vcbB = P.sb("n_vcb", [128, SP], BF16)
    ksr, ksrB = P.sb("n_ksr", [128, S], BF16)
    kwr, kwrB = P.sb("n_kwr", [128, S], BF16)
    P.op("dve", lambda v: v.memset(kcr[:, S:SP], 0.0), [], [kcrB])
    P.op("dve", lambda v: v.memset(vcb[:, S:SP], 0.0), [], [vcbB])
    P.push()
    C2, tB = P.sb("n_C2", [128, S], F32)
    S2, _ = P.sb("n_S2", [128, S], F32)
    rope_tables(P, io["pos"][0], 128, 16, C2, S2, tB, S)
    HS = 2048
    X, XB = P.sb("n_X", [128, HS], F32)
    Xs, XsB = P.sb("n_Xs", [128, HS], F32)
    t1, t1B = P.sb("n_t1", [128, HS], F32)
    P.op("dve", lambda v: v.memset(Xs[:], 0.0), [], [XsB])
    jobs = [(R_NQ + 128 * h, qr[h][0], qr[h][1]) for h in range(4)]
    jobs += [(R_KC, kcr, kcrB), (R_KS, ksr, ksrB), (R_KW, kwr, kwrB)]
    for (row, dst, dstB) in jobs:
        for hh in range(2):
            sl = slice(hh * HS, (hh + 1) * HS)
            P.dma(X[:], proj[row:row + 128, sl], pj, [XB])
            P.dma(Xs[0:16, :], proj[row + 16:row + 32, sl], pj, [XsB])
            P.dma(Xs[16:32, :], proj[row:row + 16, sl], pj, [XsB])
            tt(P, t1[:], X[:], C2[:, sl], ALU.mult, [XB, tB], [t1B])
            tt(P, X[:], Xs[:], S2[:, sl], ALU.mult, [XsB, tB], [XB])
            tt(P, dst[:, sl], t1[:], X[:], ALU.add, [t1B, XB], [dstB])
    for hh in range(2):
        sl = slice(hh * HS, (hh + 1) * HS)
        P.dma(X[:], proj[R_VC:R_VC + 128, sl], pj, [XB])
        act(P, vcb[:, sl], X[:], AF.Copy, [XB], [vcbB])
    P.pop()
    kcmpT, kcmpTB = P.sb("n_kcmpT", [128, 256], BF16)
    vcmp, vcmpB = P.sb("n_vcmp", [128, 2, 128], BF16)
    P.push()
    w1, w1B = P.sb("n_w1", [128, 2, 32, 128], BF16)
    w2, w2B = P.sb("n_w2", [128, 2, 128], BF16)
    posT, posTB = P.sb("n_posT", [128, 2, 32], BF16)
    P.dma(w1[:, 0], io["w1k"], [], [w1B], q="pool")
    P.dma(w1[:, 1], io["w1v"], [], [w1B], q="pool")
    P.dma(w2[:, 0], io["w2k"], [], [w2B], q="pool")
    P.dma(w2[:, 1], io["w2v"], [], [w2B], q="pool")
    P.dma(posT[:, 0], io["poskT"], [], [posTB], q="pool")
    P.dma(posT[:, 1], io["posvT"], [], [posTB], q="pool")
    bias, biasB = P.sb("n_bias", [128, 2], F32)
    hb, hbB = P.sb("n_hb", [128, 2, 256], BF16)
    for kv, (src, srcB) in enumerate([(kcr, kcrB), (vcb, vcbB)]):
        ps, psB = pp.get()
        for l_ in range(32):
            mm(P, ps[:, 0:1], w1[:, kv, l_, :], posT[:, kv, l_:l_ + 1], l_ == 0, l_ == 31, [w1B, posTB], [psB])
        P.op("dve", lambda v, ps=ps, kv=kv: v.tensor_copy(bias[:, kv:kv + 1], ps[:, 0:1]), [psB], [biasB])
        ps, psB = pp.get()
        for l_ in range(32):
            mm(P, ps[:, 0:256], w1[:, kv, l_, :], src[:, l_:l_ + 16 * 256:16], l_ == 0, l_ == 31, [w1B, srcB], [psB])
        act(P, hb[:, kv, :], ps[:, 0:256], AF.Gelu_apprx_tanh, [psB, biasB], [hbB], bias=bias[:, kv:kv + 1], scale=1.0)
    ps, psB = pp.get()
    mm(P, ps[:, 0:256], w2[:, 0, :], hb[:, 0, :], True, True, [w2B, hbB], [psB])
    act(P, kcmpT[:], ps[:, 0:256], AF.Copy, [psB], [kcmpTB])
    for nch in range(2):
        ps, psB = pp.get()
        mm(P, ps[:, 0:128], hb[:, 1, nch * 128:(nch + 1) * 128], w2[:, 1, :], True, True, [w2B, hbB], [psB])
        act(P, vcmp[:, nch, :], ps[:, 0:128], AF.Copy, [psB], [vcmpB])
    P.pop()
    vsw, vswB = P.sb("n_vsw", [128, 2, 32, 128], BF16)
    ptv = io["projT"].rearrange("(c p) n -> p c n", p=128)
    P.dma(vsw[:, 0], ptv[:, :, 0:128], [io["projTB"]], [vswB], q="pool")
    P.dma(vsw[:, 1], ptv[:, :, 128:256], [io["projTB"]], [vswB], q="pool")
    mcmp, mcmpB = P.sb("n_mcmp", [128, 2, S], BF16)
    Ebig, EbigB = P.sb("n_Ebig", [64, S], BF16)
    ov, ovB = P.sb("n_ov", [128, 2, 65], BF16)
    JJ, JJB = P.sb("n_JJ", [128, 64], F32)
    P.push()
    tmp, tmpB = P.sb("n_tmp", [128, S], F32)
    tmp2, tmp2B = P.sb("n_tmp2", [128, S], F32)
    for nch in range(2):
        P.op("pool", lambda g, nch=nch: g.iota(tmp[:], pattern=[[1, S]], base=-31 - 2048 * nch, channel_multiplier=-16,
                                               allow_small_or_imprecise_dtypes=True), [], [tmpB])
        ts(P, tmp[:], tmp[:], 0.0, None, ALU.is_ge, None, [tmpB], [tmpB])
        ts(P, mcmp[:, nch, :], tmp[:], 1.0, -NEG, ALU.subtract, ALU.mult, [tmpB], [mcmpB])
    P.op("pool", lambda g: g.iota(tmp[0:64, :], pattern=[[1, S]], base=0, channel_multiplier=-64,
                                  allow_small_or_imprecise_dtypes=True), [], [tmpB])
    ts(P, tmp2[0:64, :], tmp[0:64, :], 0.0, None, ALU.is_ge, None, [tmpB], [tmp2B])
    ts(P, tmp[0:64, :], tmp[0:64, :], 63.0, None, ALU.is_le, None, [tmpB], [tmpB])
    tt(P, Ebig[:], tmp[0:64, :], tmp2[0:64, :], ALU.mult, [tmpB, tmp2B], [EbigB])
    for nch in range(2):
        P.op("pool", lambda g, nch=nch: g.iota(tmp[:, 0:64], pattern=[[-4, 64]], base=128 * nch, channel_multiplier=1,
                                               allow_small_or_imprecise_dtypes=True), [], [tmpB])
        ts(P, tmp2[:, 0:64], tmp[:, 0:64], -1.0, None, ALU.is_ge, None, [tmpB], [tmp2B])
        ts(P, tmp[:, 0:64], tmp[:, 0:64], 3.0, None, ALU.is_le, None, [tmpB], [tmpB])
        tt(P, ov[:, nch, 0:64], tmp[:, 0:64], tmp2[:, 0:64], ALU.mult, [tmpB, tmp2B], [ovB])
    P.op("dve", lambda v: v.memset(ov[:, :, 64:65], 1.0), [], [ovB])
    P.op("pool", lambda g: g.iota(tmp[:, 0:64], pattern=[[1, 64]], base=0, channel_multiplier=0,
                                  allow_small_or_imprecise_dtypes=True), [], [tmpB])
    P.op("pool", lambda g: g.iota(tmp2[:, 0:1], pattern=[[0, 1]], base=0, channel_multiplier=1,
                                  allow_small_or_imprecise_dtypes=True), [], [tmp2B])
    ts(P, tmp2[:, 0:1], tmp2[:, 0:1], 64.0, None, ALU.is_ge, None, [tmp2B], [tmp2B])
    ts(P, JJ[:], tmp[:, 0:64], tmp2[:, 0:1], None, ALU.subtract, None, [tmpB, tmp2B], [JJB])
    P.pop()
    km, kmB = P.sb("n_km", [128, 24], F32)
    sqb, sqbB = P.sb("n_sqb", [128, 512], BF16)
    idx = 0
    for (src, srcB, n) in [(kcmpT, kcmpTB, 256)] + [(ksr, ksrB, 512)] * 8 + [(kwr, kwrB, 512)] * 8:
        off = 0 if n == 256 else ((idx - 1) % 8) * 512
        act(P, sqb[:, :n], src[:, off:off + n], AF.Square, [srcB], [sqbB])
        ps, psB = pp.get()
        mm(P, ps[:, :n], K.onesb[:], sqb[:, :n], True, True, [K.onesbB, sqbB], [psB])
        P.op("dve", lambda v, ps=ps, idx=idx, n=n: v.reduce_max(km[:, idx:idx + 1], ps[:, :n], AX.X), [psB], [kmB])
        idx += 1
    P.op("dve", lambda v: v.reduce_max(km[:, 23:24], km[:, 0:17], AX.X), [kmB], [kmB])
    negc = [P.sb("n_negc%d" % i, [128, S], BF16) for i in range(2)]
    t32, t32B = P.sb("n_t32", [128, 512], F32)
    for h in range(4):
        nt, ntB = negc[h // 2]
        p0 = 32 * (h % 2)
        for tl in range(8):
            sl = slice(tl * 512, (tl + 1) * 512)
            act(P, sqb[:], qr[h][0][:, sl], AF.Square, [qr[h][1]], [sqbB])
            ps, psB = pp.get()
            mm(P, ps[:], K.onesb[:], sqb[:], True, True, [K.onesbB, sqbB], [psB])
            ts(P, t32[p0:p0 + 1, :], ps[p0:p0 + 1, :], km[p0:p0 + 1, 23:24], None, ALU.mult, None, [psB, kmB], [t32B])
            act(P, t32[p0:p0 + 1, :], t32[p0:p0 + 1, :], AF.Sqrt, [t32B], [t32B])
            ts(P, nt[p0:p0 + 1, sl], t32[p0:p0 + 1, :], -1.0, None, ALU.mult, None, [t32B], [ntB])

    def shift_part(h, Gsl):
        nt, ntB = negc[h // 2]
        p0 = 32 * (h % 2)
        return (lambda kc: K.onesb[p0:p0 + 1, :], nt[p0:p0 + 1, Gsl], [K.onesbB, ntB])

    ab = AttnBufs(P, pp.acc)
    gate, gateB = P.sb("n_gate", [128, 6, 512], F32)
    oacc, oaccB = P.sb("n_oacc", [128, 2, 512], F32)
    otmp, otmpB = P.sb("n_otmp", [128, 512], F32)
    imp, impB = P.sb("n_imp", [128, 4, 64], F32)
    wk, wkB = P.sb("n_wk", [128, 6, 64], F32)
    m8, m8B = P.sb("n_m8", [128, 16], F32)
    MnegT, MnegTB = P.sb("n_MnegT", [64, 512], BF16)
    rzc, rzcB = P.sb("n_rzc", [128, 1], F32)
    state = {"first": True}

    def make_post(h, gi):
        def post(po, poB, rz, rzB):
            tt(P, rz[:], rz[:], gate[:, gi * 2 + h, :], ALU.mult, [rzB, gateB], [rzB])
            if state["first"]:
                tt(P, oacc[:, h, :], po[:], rz[:], ALU.mult, [poB, rzB], [oaccB])
            else:
                tt(P, otmp[:], po[:], rz[:], ALU.mult, [poB, rzB], [otmpB])
                tt(P, oacc[:, h, :], oacc[:, h, :], otmp[:], ALU.add, [oaccB, otmpB], [oaccB])
        return post

    for G in range(8):
        Gsl = slice(G * 512, (G + 1) * 512)
        for gi in range(6):
            P.dma(gate[:, gi, :], proj[R_NG + gi, Gsl].partition_broadcast(128), pj, [gateB])
        act(P, gate[:], gate[:], AF.Sigmoid, [gateB], [gateB])
        kcs = [0] if G < 4 else [0, 1]
        for h in range(4):
            (po, poB), (pz, pzB) = ab.acc
            pTs = []
            sp_ = shift_part(h, Gsl)
            for i, kc in enumerate(kcs):
                ps, psB = pp.get()
                mm(P, ps[:], kcmpT[:, kc * 128:(kc + 1) * 128], qr[h][0][:, Gsl], True, False, [kcmpTB, qr[h][1]], [psB])
                mm(P, ps[:], sp_[0](kc), sp_[1], False, False, sp_[2], [psB])
                mm(P, ps[:], K.identb[:], mcmp[:, kc, Gsl], False, True, [K.identbB, mcmpB], [psB])
                pT, pTB = ab.next_pT()
                act(P, pT[:], ps[:], AF.Exp, [psB], [pTB], scale=SCALE)
                pTs.append((pT, pTB))
                if h < 2:
                    mm(P, po[:], vcmp[:, kc, :], pT[:], i == 0, i == len(kcs) - 1, [vcmpB, pTB], [poB])
                    mm(P, pz[:], K.onesb[:], pT[:], i == 0, i == len(kcs) - 1, [K.onesbB, pTB], [pzB])
            pim, pimB = pp.get()
            for tcn in range(4):
                for i, kc in enumerate(kcs):
                    mm(P, pim[:, tcn * 65:(tcn + 1) * 65], pTs[i][0][:, tcn * 128:(tcn + 1) * 128], ov[:, kc, :],
                       i == 0, i == len(kcs) - 1, [pTs[i][1], ovB], [pimB])
            for tcn in range(4):
                ts(P, rzc[:], pim[:, tcn * 65 + 64:tcn * 65 + 65], 1e-30, None, ALU.max, None, [pimB], [rzcB])
                P.op("dve", lambda v: v.reciprocal(rzc[:], rzc[:]), [rzcB], [rzcB])
                if h == 0:
                    ts(P, imp[:, tcn, :], pim[:, tcn * 65:tcn * 65 + 64], rzc[:, 0:1], None, ALU.mult, None,
                       [pimB, rzcB], [impB])
                else:
                    stt(P, imp[:, tcn, :], pim[:, tcn * 65:tcn * 65 + 64], rzc[:, 0:1], imp[:, tcn, :], ALU.mult, ALU.add,
                        [pimB, rzcB, impB], [impB])
            if h < 2:
                state["first"] = True
                ts(P, ab.rz[:], pz[:], 1e-30, None, ALU.max, None, [pzB], [ab.rzB])
                P.op("dve", lambda v: v.reciprocal(ab.rz[:], ab.rz[:]), [ab.rzB], [ab.rzB])
                make_post(h, 0)(po, poB, ab.rz, ab.rzB)
        state["first"] = False
        if G >= 2:
            for tcn in range(4):
                i2 = float(2 * (4 * G + tcn))
                keep, fut, impo, w2_, sel, mneg = (wk[:, k_, :] for k_ in range(6))
                ts(P, keep, JJ[:], i2, -1.0, ALU.subtract, ALU.is_lt, [JJB], [wkB])
                ts(P, fut, JJ[:], i2, 0.0, ALU.subtract, ALU.is_gt, [JJB], [wkB])
                tt(P, impo, imp[:, tcn, :], keep, ALU.mult, [impB, wkB], [wkB])
                ts(P, w2_, keep, -1e6, 1e6, ALU.mult, ALU.add, [wkB], [wkB])
                tt(P, impo, impo, w2_, ALU.add, [wkB], [wkB])
                stt(P, impo, fut, -2e6, impo, ALU.mult, ALU.add, [wkB], [wkB])
                P.op("dve", lambda v: v.memset(wk[:, 2, 0:1], 1e6), [wkB], [wkB])
                P.op("dve", lambda v: v.max(out=m8[:, 0:8], in_=wk[:, 2, :]), [wkB], [m8B])
                P.op("dve", lambda v: v.match_replace(out=wk[:, 3, :], in_to_replace=m8[:, 0:8], in_values=wk[:, 2, :],
                                                      imm_value=-3e38), [wkB, m8B], [wkB])
                P.op("dve", lambda v: v.max(out=m8[:, 8:16], in_=wk[:, 3, :]), [wkB], [m8B])
                ts(P, sel, impo, m8[:, 15:16], None, ALU.is_ge, None, [wkB, m8B], [wkB])
                ts(P, mneg, sel, 1.0, -NEG, ALU.subtract, ALU.mult, [wkB], [wkB])
                ps, psB = pp.get()
                P.op("pe", lambda t, ps=ps: t.transpose(ps[0:64, 0:128], wk[:, 5, :], K.identf[:]), [wkB, K.identfB], [psB])
                act(P, MnegT[:, tcn * 128:(tcn + 1) * 128], ps[0:64, 0:128], AF.Copy, [psB], [MnegTB])
        for h in range(2):
            sp_ = shift_part(h, Gsl)
            kcs = list(range(4 * G + 4))
            parts = [(lambda kc: ksr[:, kc * 128:(kc + 1) * 128], qr[h][0][:, Gsl], [ksrB, qr[h][1]]), sp_]
            extra = None
            if G >= 2:
                extra = lambda kc: (Ebig[:, kc * 128:(kc + 1) * 128], MnegT[:], [EbigB, MnegTB])
            attention_group(P, pp, K, ab, G, kcs, parts, lambda kc: vsw[:, 0, kc, :], vswB, SCALE, None, None,
                            lambda kc, G=G: (kc - 4 * G) if kc >= 4 * G else None, extra=extra, post=make_post(h, 1))
            kcs = list(range(max(0, 4 * G - 4), 4 * G + 4))
            parts = [(lambda kc: kwr[:, kc * 128:(kc + 1) * 128], qr[h][0][:, Gsl], [kwrB, qr[h][1]]), sp_]
            attention_group(P, pp, K, ab, G, kcs, parts, lambda kc: vsw[:, 1, kc, :], vswB, SCALE, None, None,
                            lambda kc, G=G: 4 + (kc - 4 * G + 4), post=make_post(h, 2))
        P.dma(io["o_nsa"][:, Gsl].rearrange("(h p) t -> p h t", p=128), oacc[:], [oaccB], [io["oB"]], is_output=True)


def _declare(nc, hin):
    io = {}
    for k, v in hin.items():
        dt = I32 if v.dtype == np.int32 else F32
        io[k] = nc.dram_tensor(k, list(v.shape), dt, kind="ExternalInput").ap()
    return io


def build_ada(hin):
    nc = bass.Bass("TRN2", target_bir_lowering=False)
    io = _declare(nc, hin)
    outp = nc.dram_tensor("modsP", [128, 2, 12, 4], F32, kind="ExternalOutput").ap()
    P = Prog(nc)
    pp = PsumPool(P)
    cT, cB = P.sb("d_cT", [128, 16, 4], F32)
    P.dma(cT[:], io["cT4"], [], [cB])
    cond, condB = P.sb("d_cond", [128, 16, 4], F32)
    act(P, cond[:], cT[:], AF.Silu, [cB], [condB])
    bT, bB = P.sb("d_bT", [128, 2, 12], F32)
    P.dma(bT[:], io["bT"], [], [bB])
    res, resB = P.sb("d_res", [128, 2, 12, 4], F32)
    wa = [P.sb("d_wa%d" % i, [128, 16, 512], F32) for i in range(2)]
    k = 0
    for l in range(2):
        for g in range(3):
            wt, wB = wa[k % 2]
            P.dma(wt[:], io["w"][l, g], [], [wB], q="sp" if k % 2 == 0 else "act")
            k += 1
            for j in range(4):
                ps, psB = pp.get()
                for kc in range(16):
                    mm(P, ps[:, 0:4], wt[:, kc, j * 128:(j + 1) * 128], cond[:, kc, :], kc == 0, kc == 15,
                       [wB, condB], [psB])
                ts(P, res[:, l, g * 4 + j, :], ps[:, 0:4], bT[:, l, g * 4 + j:g * 4 + j + 1], None, ALU.add, None,
                   [psB, bB], [resB])
    P.dma(outp, res[:], [resB], [], is_output=True)
    P.finish()
    return nc


def ada_host_inputs(inp, c):
    cols = slice(c * 1536, (c + 1) * 1536)
    w = np.stack([inp["w_ada"][l][:, cols].reshape(16, 128, 3, 512).transpose(2, 1, 0, 3) for l in range(2)])
    bT = np.stack([inp["b_ada"][l][cols].reshape(12, 128).T for l in range(2)], axis=1)
    cT4 = inp["c"].reshape(NB, 16, 128).transpose(2, 1, 0)
    return {"cT4": np.ascontiguousarray(cT4.astype(np.float32)), "w": np.ascontiguousarray(w.astype(np.float32)),
            "bT": np.ascontiguousarray(bT.astype(np.float32))}


def mix_host_inputs(inp, l, b, hf, xT):
    hin = {}
    for pre, d in (("a_", host_phase_a_inputs(inp, l, b, hf)), ("mla_", host_mla_inputs(inp, l, b, hf)),
                   ("s5_", host_s5_inputs(inp, l, hf)), ("nsa_", host_nsa_inputs(inp, l, b, hf)),
                   ("gdn_", host_gdn_inputs(inp, l, hf))):
        for k, v in d.items():
            hin[pre + k] = v
    hin["a_xT"] = xT
    return hin


def run_ada(inp):
    in_maps = [ada_host_inputs(inp, c) for c in range(8)]
    nc = build_ada(in_maps[0])
    res = run_bass_kernel_spmd(nc, in_maps, core_ids=list(range(8))).results
    out = [[np.zeros((128, 96), np.float32) for _ in range(NB)] for _ in range(2)]
    for c in range(8):
        mp = res[c]["modsP"]
        for l in range(2):
            for b in range(NB):
                out[l][b][:, c * 12:(c + 1) * 12] = mp[:, l, :, b]
    return out


def build_mix(hin, which=("mla", "s5", "nsa", "gdn")):
    nc = bass.Bass("TRN2", target_bir_lowering=False)
    io = _declare(nc, hin)
    proj = nc.dram_tensor("proj", [NROWS, S], F32).ap()
    projT = nc.dram_tensor("projT", [S, NTM], F32).ap()
    o_core = nc.dram_tensor("o_core", [1024, S], F32, kind="ExternalOutput").ap()
    P = Prog(nc)
    pp = PsumPool(P)
    base = dict(proj=proj, projT=projT, projB=P.buf("proj"), projTB=P.buf("projT"), oB=P.buf("o"),
                o_mla=o_core[0:256], o_s5=o_core[256:512], o_nsa=o_core[512:768], o_gdn=o_core[768:1024])

    def sub(pre):
        d = dict(base)
        for k in io:
            if k.startswith(pre):
                d[k[len(pre):]] = io[k]
        return d

    P.push()
    phase_a(P, pp, sub("a_"), NCOLCH, NTM)
    P.pop()
    for name, fn in (("mla", mixer_mla), ("s5", mixer_s5), ("nsa", mixer_nsa), ("gdn", mixer_gdn)):
        if name in which:
            P.push()
            fn(P, pp, Consts(P), sub(name + "_"))
            P.pop()
    P.finish()
    return nc


def post_host_inputs(inp, l, th, oT, xT, modsT):
    hin = host_phase_b_inputs(inp, l)
    t0 = th * 2048

    def halo(a):
        if t0 == 0:
            return np.ascontiguousarray(np.concatenate([np.zeros((a.shape[0], 2), a.dtype), a[:, 0:2048]], axis=1))
        return np.ascontiguousarray(a[:, t0 - 2:t0 + 2048])

    hin["oT"] = halo(oT)
    hin["xin"] = halo(xT)
    hin["modsT"] = modsT
    hin["halo"] = np.full((128, 1), 1.0 if th == 1 else 0.0, np.float32)
    return hin


def build_post(hin):
    nc = bass.Bass("TRN2", target_bir_lowering=False)
    io = _declare(nc, hin)
    io["xmid"] = nc.dram_tensor("xmid", [D, WB], F32).ap()
    io["xout"] = nc.dram_tensor("xout", [D, 2048], F32, kind="ExternalOutput").ap()
    P = Prog(nc)
    io["xmidB"] = P.buf("xmid")
    pp = PsumPool(P)
    phase_b(P, pp, io)
    P.finish()
    return nc


def fused_host_inputs(inp, b):
    hin = {}
    xT = np.ascontiguousarray(inp["x"][b].T.astype(np.float32))
    hin["xpad"] = np.ascontiguousarray(np.concatenate([np.zeros((D, 2), np.float32), xT], axis=1))
    hin["cT"] = vecT(inp["c"][b])
    hin["halo"] = np.zeros((128, 1), np.float32)
    for l in range(2):
        hin["L%d_w_ada" % l] = np.ascontiguousarray(inp["w_ada"][l].reshape(16, 128, 24, 512).transpose(2, 1, 0, 3))
        hin["L%d_b_adaT" % l] = vecT(inp["b_ada"][l])
        for hf in range(2):
            for pre, d in (("a_", host_phase_a_inputs(inp, l, b, hf)), ("mla_", host_mla_inputs(inp, l, b, hf)),
                           ("s5_", host_s5_inputs(inp, l, hf)), ("nsa_", host_nsa_inputs(inp, l, b, hf)),
                           ("gdn_", host_gdn_inputs(inp, l, hf))):
                for k, v in d.items():
                    if v is not None:
                        hin["L%dH%d_%s%s" % (l, hf, pre, k)] = v
        for k, v in host_phase_b_inputs(inp, l).items():
            hin["L%d_b_%s" % (l, k)] = v
    return hin


def build_fused(hin, nlayers=2):
    nc = bass.Bass("TRN2", target_bir_lowering=False)
    io = _declare(nc, hin)
    proj = nc.dram_tensor("proj", [NROWS, S], F32).ap()
    projT = nc.dram_tensor("projT", [S, NTM], F32).ap()
    oTf = nc.dram_tensor("oT_full", [D, 2 + S], F32).ap()
    xn0 = nc.dram_tensor("xn0", [D, 2 + S], F32).ap()
    xmid = nc.dram_tensor("xmid", [D, WB], F32).ap()
    modsD = [nc.dram_tensor("modsD%d" % l, [128, 96], F32).ap() for l in range(2)]
    xfinal = nc.dram_tensor("xfinal", [D, S], F32, kind="ExternalOutput").ap()
    P = Prog(nc)
    pp = PsumPool(P)
    zt, ztB = P.sb("f_zero", [128, 16, 2], F32)
    P.op("dve", lambda v: v.memset(zt[:], 0.0), [], [ztB])
    for t_ in (oTf, xn0):
        P.dma(t_.rearrange("(kc p) t -> p kc t", p=128)[:, :, 0:2], zt[:], [ztB], [])
    P.barrier()
    base = dict(proj=proj, projT=projT, projB=P.buf("proj"), projTB=P.buf("projT"), oB=P.buf("o"))

    def sub(pre, extra=None):
        d = dict(base)
        for k in io:
            if k.startswith(pre):
                d[k[len(pre):]] = io[k]
        if extra:
            d.update(extra)
        return d

    for l in range(nlayers):
        xsrc = io["xpad"] if l == 0 else xn0
        for hf in range(2):
            pre = "L%dH%d_" % (l, hf)
            ex = {"xT": xsrc[:, 2:2 + S]}
            if hf == 0:
                ex.update(cT=io["cT"], w_ada=io["L%d_w_ada" % l], b_adaT=io["L%d_b_adaT" % l], modsT=modsD[l])
            else:
                ex.update(modsT_in=modsD[l])
            P.push()
            phase_a(P, pp, sub(pre + "a_", ex), NCOLCH, NTM)
            P.pop()
            for mi, (name, fn) in enumerate((("mla", mixer_mla), ("s5", mixer_s5), ("nsa", mixer_nsa), ("gdn", mixer_gdn))):
                r0 = mi * 512 + hf * 256
                ex2 = {"o_" + name: oTf[r0:r0 + 256, 2:2 + S]}
                P.push()
                fn(P, pp, Consts(P), sub(pre + name + "_", ex2))
                P.pop()
        last = (l == nlayers - 1)
        exb = dict(oT=oTf, xin=xsrc, modsT=modsD[l], halo=io["halo"], xmid=xmid, xmidB=P.buf("xmid"),
                   xout=(xfinal if last else xn0[:, 2:2 + S]))
        P.push()
        phase_b(P, pp, sub("L%d_b_" % l, exb), ntiles=4)
        P.pop()
    P.finish()
    return nc


FUSED = False


def kernel(**inp):
    inp = {k: np.asarray(v) for k, v in inp.items()}
    if FUSED:
        in_maps = [fused_host_inputs(inp, b) for b in range(NB)]
        nc = build_fused(in_maps[0])
        res = run_bass_kernel_spmd(nc, in_maps, core_ids=list(range(NB))).results
        return np.ascontiguousarray(np.stack([res[b]["xfinal"].T for b in range(NB)])).astype(np.float32)
    return kernel_unfused(inp)


def kernel_unfused(inp):
    x = inp["x"].astype(np.float32)
    xT = [np.ascontiguousarray(x[b].T) for b in range(NB)]
    cores = [(b, hf) for b in range(NB) for hf in range(2)]
    mods_all = run_ada(inp)
    for l in range(2):
        modsT = mods_all[l]
        in_maps = [mix_host_inputs(inp, l, b, hf, xT[b]) for (b, hf) in cores]
        for i, (b, hf) in enumerate(cores):
            in_maps[i]["a_modsT_in"] = modsT[b]
        nc = build_mix(in_maps[0])
        res = run_bass_kernel_spmd(nc, in_maps, core_ids=list(range(8))).results
        oT = []
        for b in range(NB):
            r0, r1 = res[2 * b]["o_core"], res[2 * b + 1]["o_core"]
            oT.append(np.concatenate([np.concatenate([r0[m * 256:(m + 1) * 256], r1[m * 256:(m + 1) * 256]], axis=0)
                                      for m in range(4)], axis=0))
        del res, in_maps
        in_maps = [post_host_inputs(inp, l, th, oT[b], xT[b], modsT[b]) for (b, th) in cores]
        nc = build_post(in_maps[0])
        res = run_bass_kernel_spmd(nc, in_maps, core_ids=list(range(8))).results
        xT = [np.ascontiguousarray(np.concatenate([res[2 * b]["xout"], res[2 * b + 1]["xout"]], axis=1))
              for b in range(NB)]
        del res, in_maps
    return np.ascontiguousarray(np.stack([xT[b].T for b in range(NB)])).astype(np.float32)
```
